# Optimizing a Trainium2 kernel written in Bass

```python
import jax, jax.numpy as jnp
from jax import lax
import numpy as np

D_MODEL = 2048
BATCH = 4
SEQ = 2048
DEPTH = 1

HEAD_DIM = 64
SWA_Q_HEADS = 16
SWA_KV_HEADS = 4
SWA_GROUP = SWA_Q_HEADS // SWA_KV_HEADS
WINDOW = 128
FOX_HEADS = 16
BLOCK_Q = 128
ROPE_THETA = 10000.0
FGATE_BIAS_INIT = 3.0

N_EXPERTS = 32
TOP_K = 4
D_EXPERT = D_MODEL
SWIGLU_LIMIT = 7.0
SWIGLU_ALPHA = 1.702
MOE_BLOCK = 128

RMS_EPS = 1e-5
NEG_INF = -1e30

SWA_Q_W = SWA_Q_HEADS * HEAD_DIM
SWA_KV_W = SWA_KV_HEADS * HEAD_DIM
FOX_W = FOX_HEADS * HEAD_DIM
IN_WIDTHS = [SWA_Q_W, SWA_KV_W, SWA_KV_W, FOX_W, FOX_W, FOX_W, FOX_HEADS, D_MODEL, D_MODEL]
IN_W = sum(IN_WIDTHS)
IN_SPLITS = [int(v) for v in np.cumsum(IN_WIDTHS)[:-1]]
FGATE_OFFSET = SWA_Q_W + 2 * SWA_KV_W + 3 * FOX_W

kernel_name = "hybrid_swa_sink_fox_gated_moe_adaln"


def rms_norm(x, g):
    xf = x.astype(jnp.float32)
    y = xf * lax.rsqrt(jnp.mean(xf * xf, axis=-1, keepdims=True) + RMS_EPS)
    return (y * g.astype(jnp.float32)).astype(x.dtype)


def rope(x, positions):
    hd = x.shape[-1]
    inv_freq = 1.0 / (ROPE_THETA ** (jnp.arange(0, hd, 2, dtype=jnp.float32) / hd))
    ang = positions.astype(jnp.float32)[..., None] * inv_freq
    cos = jnp.cos(ang)[:, :, None, :]
    sin = jnp.sin(ang)[:, :, None, :]
    xf = x.astype(jnp.float32)
    x1, x2 = jnp.split(xf, 2, axis=-1)
    out = jnp.concatenate([x1 * cos - x2 * sin, x2 * cos + x1 * sin], axis=-1)
    return out.astype(x.dtype)


def swa_sink_attention(q, k, v, sinks):
    B, S, _, hd = q.shape
    W = WINDOW
    nb = S // W
    scale = hd ** -0.5
    qb = q.reshape(B, nb, W, SWA_KV_HEADS, SWA_GROUP, hd)
    kb = k.reshape(B, nb, W, SWA_KV_HEADS, hd)
    vb = v.reshape(B, nb, W, SWA_KV_HEADS, hd)
    pad = ((0, 0), (1, 0), (0, 0), (0, 0), (0, 0))
    kk = jnp.concatenate([jnp.pad(kb, pad)[:, :-1], kb], axis=2)
    vv = jnp.concatenate([jnp.pad(vb, pad)[:, :-1], vb], axis=2)
    scores = jnp.einsum('bnqhgd,bnkhd->bnhgqk', qb, kk).astype(jnp.float32) * scale
    qi = jnp.arange(W)[:, None]
    kj = jnp.arange(2 * W)[None, :]
    delta = qi + W - kj
    band = (delta >= 0) & (delta < WINDOW)
    key_pos = jnp.arange(nb)[:, None, None] * W - W + kj[None]
    valid = band[None] & (key_pos >= 0)
    scores = jnp.where(valid[None, :, None, None], scores, NEG_INF)
    sink_col = jnp.broadcast_to(
        sinks.astype(jnp.float32).reshape(1, 1, SWA_KV_HEADS, SWA_GROUP, 1, 1),
        scores.shape[:-1] + (1,))
    probs = jax.nn.softmax(jnp.concatenate([scores, sink_col], axis=-1), axis=-1)[..., :-1]
    out = jnp.einsum('bnhgqk,bnkhd->bnqhgd', probs.astype(v.dtype), vv)
    return out.reshape(B, S, SWA_Q_HEADS * hd)


def forgetting_attention(q, k, v, log_f):
    B, S, H, hd = q.shape
    nb = S // BLOCK_Q
    scale = hd ** -0.5
    cum = jnp.transpose(jnp.cumsum(log_f, axis=1), (0, 2, 1))
    q_blocks = jnp.transpose(q.reshape(B, nb, BLOCK_Q, H, hd), (1, 0, 2, 3, 4))
    cum_q = jnp.transpose(cum.reshape(B, H, nb, BLOCK_Q), (2, 0, 1, 3))
    starts = jnp.arange(nb, dtype=jnp.int32) * BLOCK_Q
    key_pos = jnp.arange(S, dtype=jnp.int32)

    def one_block(args):
        qb, cq, start = args
        s = jnp.einsum('bqhd,bkhd->bhqk', qb, k).astype(jnp.float32) * scale
        s = s + cq[..., :, None] - cum[:, :, None, :]
        q_pos = start + jnp.arange(BLOCK_Q, dtype=jnp.int32)
        causal = key_pos[None, :] <= q_pos[:, None]
        p = jax.nn.softmax(jnp.where(causal, s, NEG_INF), axis=-1)
        return jnp.einsum('bhqk,bkhd->bqhd', p.astype(v.dtype), v)

    out = lax.map(one_block, (q_blocks, cum_q, starts))
    return jnp.transpose(out, (1, 0, 2, 3, 4)).reshape(B, S, H * hd)


def hybrid_mixer(xn, positions, w_in, b_in, sinks, w_branch_a, w_branch_b, w_out):
    B, S, _ = xn.shape
    proj = xn @ w_in + b_in
    qa, ka, va, qb, kb, vb, fb, ga, gb = jnp.split(proj, IN_SPLITS, axis=-1)
    qa = rope(qa.reshape(B, S, SWA_Q_HEADS, HEAD_DIM), positions)
    ka = rope(ka.reshape(B, S, SWA_KV_HEADS, HEAD_DIM), positions)
    va = va.reshape(B, S, SWA_KV_HEADS, HEAD_DIM)
    ya = swa_sink_attention(qa, ka, va, sinks)
    log_f = jax.nn.log_sigmoid(fb.astype(jnp.float32))
    yb = forgetting_attention(qb.reshape(B, S, FOX_HEADS, HEAD_DIM),
                              kb.reshape(B, S, FOX_HEADS, HEAD_DIM),
                              vb.reshape(B, S, FOX_HEADS, HEAD_DIM), log_f)
    merged = jax.nn.sigmoid(ga) * (ya @ w_branch_a) + jax.nn.sigmoid(gb) * (yb @ w_branch_b)
    return merged @ w_out


def moe_ffn(xn, w_router, b_router, w_gu, b_gu, w_down, b_down):
    B, S, D = xn.shape
    N = B * S
    A = N * TOP_K
    xf = xn.reshape(N, D)
    logits = (xf @ w_router + b_router).astype(jnp.float32)
    top_val, top_idx = lax.top_k(logits, TOP_K)
    gates = jax.nn.softmax(top_val, axis=-1)
    eid = top_idx.reshape(A).astype(jnp.int32)
    tok = jnp.arange(A, dtype=jnp.int32) // TOP_K
    gw = gates.reshape(A)
    order = jnp.argsort(eid)
    s_eid, s_tok, s_w = eid[order], tok[order], gw[order]
    counts = jnp.bincount(eid, length=N_EXPERTS).astype(jnp.int32)
    starts = jnp.cumsum(counts) - counts
    pad_counts = ((counts + MOE_BLOCK - 1) // MOE_BLOCK) * MOE_BLOCK
    pad_ends = jnp.cumsum(pad_counts)
    pad_starts = pad_ends - pad_counts
    dest = pad_starts[s_eid] + (jnp.arange(A, dtype=jnp.int32) - starts[s_eid])
    P = A + N_EXPERTS * MOE_BLOCK
    n_blocks = P // MOE_BLOCK
    row_tok = jnp.zeros((P,), jnp.int32).at[dest].set(s_tok)
    row_w = jnp.zeros((P,), jnp.float32).at[dest].set(s_w)
    block_starts = jnp.arange(n_blocks, dtype=jnp.int32) * MOE_BLOCK
    block_e = jnp.minimum(jnp.searchsorted(pad_ends, block_starts, side='right'),
                          N_EXPERTS - 1).astype(jnp.int32)
    x_rows = xf[row_tok].reshape(n_blocks, MOE_BLOCK, D)

    def expert_block(args):
        xb, e = args
        hcat = xb @ w_gu[e] + b_gu[e]
        glu, lin = jnp.split(hcat, 2, axis=-1)
        glu = jnp.minimum(glu, SWIGLU_LIMIT)
        lin = jnp.clip(lin, -SWIGLU_LIMIT, SWIGLU_LIMIT)
        act = glu * jax.nn.sigmoid(SWIGLU_ALPHA * glu) * (lin + 1.0)
        return act @ w_down[e] + b_down[e]

    y_rows = lax.map(expert_block, (x_rows, block_e)).reshape(P, D)
    y = jax.ops.segment_sum(y_rows * row_w[:, None].astype(y_rows.dtype), row_tok,
                            num_segments=N)
    return y.reshape(B, S, D)


def setup_inputs(seed: int = 0) -> dict:
    key = jax.random.key(seed)
    ks = jax.random.split(key, 20)
    f32 = jnp.float32
    D, F, E, L = D_MODEL, D_EXPERT, N_EXPERTS, DEPTH
    x = jax.random.normal(ks[0], (BATCH, SEQ, D), f32)
    c = jax.random.normal(ks[1], (BATCH, D), f32)
    positions = jnp.broadcast_to(jnp.arange(SEQ, dtype=jnp.int32)[None, :], (BATCH, SEQ))
    w_ada = 0.5 * jax.random.normal(ks[2], (L, D, 6 * D), f32) * D ** -0.5
    b_ada = 0.02 * jax.random.normal(ks[3], (L, 6 * D), f32)
    g_mix = 1.0 + 0.02 * jax.random.normal(ks[4], (L, D), f32)
    w_in = jax.random.normal(ks[5], (L, D, IN_W), f32) * D ** -0.5
    b_in = 0.01 * jax.random.normal(ks[6], (L, IN_W), f32)
    b_in = b_in.at[:, FGATE_OFFSET:FGATE_OFFSET + FOX_HEADS].add(FGATE_BIAS_INIT)
    sinks = 0.5 * jax.random.normal(ks[7], (L, SWA_Q_HEADS), f32)
    w_branch_a = jax.random.normal(ks[8], (L, SWA_Q_W, D), f32) * SWA_Q_W ** -0.5
    w_branch_b = jax.random.normal(ks[9], (L, FOX_W, D), f32) * FOX_W ** -0.5
    w_out = jax.random.normal(ks[10], (L, D, D), f32) * D ** -0.5
    g_ffn = 1.0 + 0.02 * jax.random.normal(ks[11], (L, D), f32)
    w_router = jax.random.normal(ks[12], (L, D, E), f32) * D ** -0.5
    b_router = 0.01 * jax.random.normal(ks[13], (L, E), f32)
    w_gu = jax.random.normal(ks[14], (L, E, D, 2 * F), f32) * D ** -0.5
    b_gu = 0.01 * jax.random.normal(ks[15], (L, E, 2 * F), f32)
    w_down = jax.random.normal(ks[16], (L, E, F, D), f32) * F ** -0.5
    b_down = 0.01 * jax.random.normal(ks[17], (L, E, D), f32)
    g_final = 1.0 + 0.02 * jax.random.normal(ks[18], (D,), f32)
    return {"x": x, "c": c, "positions": positions, "w_ada": w_ada, "b_ada": b_ada,
            "g_mix": g_mix, "w_in": w_in, "b_in": b_in, "sinks": sinks,
            "w_branch_a": w_branch_a, "w_branch_b": w_branch_b, "w_out": w_out,
            "g_ffn": g_ffn, "w_router": w_router, "b_router": b_router,
            "w_gu": w_gu, "b_gu": b_gu, "w_down": w_down, "b_down": b_down,
            "g_final": g_final}


def reference(x, c, positions, w_ada, b_ada, g_mix, w_in, b_in, sinks, w_branch_a,
              w_branch_b, w_out, g_ffn, w_router, b_router, w_gu, b_gu, w_down, b_down,
              g_final):
    h = x
    c_act = jax.nn.silu(c)
    for l in range(DEPTH):
        mod = c_act @ w_ada[l] + b_ada[l]
        sh1, sc1, gt1, sh2, sc2, gt2 = [m[:, None, :] for m in jnp.split(mod, 6, axis=-1)]
        xn = rms_norm(h, g_mix[l]) * (1.0 + sc1) + sh1
        h = h + gt1 * hybrid_mixer(xn, positions, w_in[l], b_in[l], sinks[l],
                                   w_branch_a[l], w_branch_b[l], w_out[l])
        xn = rms_norm(h, g_ffn[l]) * (1.0 + sc2) + sh2
        h = h + gt2 * moe_ffn(xn, w_router[l], b_router[l], w_gu[l], b_gu[l],
                              w_down[l], b_down[l])
    return rms_norm(h, g_final)
```

```python
import os
from contextlib import ExitStack
import numpy as np
import concourse.bass as bass
import concourse.mybir as mybir
from concourse.bass_utils import run_bass_kernel_spmd

F32 = mybir.dt.float32
BF16 = mybir.dt.bfloat16
I32 = mybir.dt.int32
AF = mybir.ActivationFunctionType
ALU = mybir.AluOpType
AX = mybir.AxisListType

D = 2048
NE = 32
FG = 1024 + 256 + 256 + 3 * 1024
GA0 = FG + 16
GB0 = GA0 + 2048
NEG = -30000.0
COMPUTE = ("pe", "act", "dve", "pool")
STAGE = int(os.environ.get("MK_STAGE", "99"))
NOCC = os.environ.get("MK_NOCC", "0") == "1"


class Sched:
    def __init__(self, nc, n_dma_sems=32, same_engine_sync=True):
        self.nc = nc
        self.ops = {e: [] for e in ("pe", "act", "dve", "pool", "sp")}
        self.count = {e: 0 for e in COMPUTE}
        self.res = {}
        self.seen = {e: {} for e in self.ops}
        self.n_dma = n_dma_sems
        self.dma_use = [0] * n_dma_sems
        self.dma_next = 0
        self.same = same_engine_sync
        self.sems = {}
        self.final = []

    @staticmethod
    def _need(need, key, val):
        if val > need.get(key, 0):
            need[key] = val

    def op(self, eng, fn, reads=(), writes=(), dma=False, cc=False):
        need = {}
        for r in reads:
            st = self.res.get(r)
            if st and st["w"]:
                self._need(need, *st["w"])
        for r in writes:
            st = self.res.get(r)
            if st:
                if st["w"]:
                    self._need(need, *st["w"])
                for k, v in st["r"].items():
                    self._need(need, k, v)
        if cc:
            self.n_cc = getattr(self, "n_cc", 0) + 1
            me = (("cc", self.n_cc), 1)
        elif dma:
            s = self.dma_next
            self.dma_next = (s + 1) % self.n_dma
            if self.dma_use[s] > 0:
                self._need(need, ("dma", s), self.dma_use[s] * 16)
            self.dma_use[s] += 1
            me = (("dma", s), self.dma_use[s] * 16)
        else:
            self.count[eng] += 1
            me = (eng, self.count[eng])
        waits = []
        for k, v in need.items():
            if k == eng and not dma and not cc:
                if eng == "pe" or not self.same:
                    continue
            if self.seen[eng].get(k, 0) >= v:
                continue
            self.seen[eng][k] = v
            waits.append((k, v))
        self.ops[eng].append((waits, fn, me))
        for r in reads:
            st = self.res.setdefault(r, {"w": None, "r": {}})
            st["r"][me[0]] = max(st["r"].get(me[0], 0), me[1])
        for r in writes:
            self.res[r] = {"w": me, "r": {}}
        return me

    def finish(self, eng, deps):
        self.final.append((eng, list(deps)))

    def emit(self, es):
        nc = self.nc
        keys = list(COMPUTE) + [("dma", i) for i in range(self.n_dma)] + [("cc", i + 1) for i in range(getattr(self, "n_cc", 0))]
        for k in keys:
            nm = "s_" + (k if isinstance(k, str) else "%s%d" % (k[0][0], k[1]))
            self.sems[k] = es.enter_context(nc.semaphore(nm))
        block = es.enter_context(nc.Block())
        sems = self.sems

        def run(engname):
            def body(e):
                for waits, fn, me in self.ops[engname]:
                    for k, v in waits:
                        e.wait_ge(sems[k], v)
                    ins = fn(e)
                    if isinstance(me[0], tuple) and me[0][0] == "cc":
                        ins.then_inc(sems[me[0]])
                    else:
                        ins.then_inc(sems[me[0]], 16 if isinstance(me[0], tuple) else 1)
                for en, deps in self.final:
                    if en == engname:
                        for k, v in deps:
                            e.wait_ge(sems[k], v)
            return body

        block.tensor(run("pe"))
        block.scalar(run("act"))
        block.vector(run("dve"))
        block.gpsimd(run("pool"))
        block.sync(run("sp"))


def build_program():
    nc = bass.Bass("TRN2", target_bir_lowering=False)

    def din(name, shape, dt=F32):
        return nc.dram_tensor(name, list(shape), dt, kind="ExternalInput").ap()

    xw_own = din("xw_own", [1024, D])
    xw_oth = din("xw_oth", [1024, D])
    posb = din("posb", [1, 2048], I32)
    fake = din("fake", [128, 1])
    oh8d = din("oh8", [128, 8])
    ohbd = din("ohb", [128, 4])
    cT4 = din("cT4", [128, 64])
    w_ada_s = din("w_ada_s", [D, 1536])
    b_ada_s = din("b_ada_s", [1, 1536])
    gmT = din("gmT", [128, 16])
    gfT = din("gfT", [128, 16])
    gfin = din("gfin", [1, D])
    w_in = din("w_in", [D, 8720])
    b_in = din("b_in", [1, 8720])
    w_sq = din("w_sq", [D, 1024])
    w_sqr = din("w_sqr", [D, 1024])
    w_skr = din("w_skr", [D, 256])
    bcols = din("bcols", [128, 80])
    sinks = din("sinks", [1, 16])
    w_ba = din("w_ba", [1024, D])
    w_bb = din("w_bb", [1024, D])
    w_out = din("w_out", [D, D])
    w_r = din("w_r", [D, NE])
    b_r = din("b_r", [1, NE])
    b_dn = din("b_dn", [NE, D])
    if STAGE >= 8:
        w_gu = din("w_gu_l", [4, D, 2 * D])
        b_guT = din("b_guT_l", [128, 4 * 32])
        w_dn = din("w_dn_l", [4, D, D])
    out = nc.dram_tensor("out", [1024, D], F32, kind="ExternalOutput").ap()
    modg = nc.dram_tensor("modg", [4, 6 * D], F32).ap()
    moda = din("moda_dbg", [4, 6 * D]) if NOCC else nc.dram_tensor("moda", [4, 6 * D], F32).ap()
    if STAGE >= 8:
        xg = nc.dram_tensor("xg", [1024, 8192], F32)
        xga = nc.dram_tensor("xga", [1024, 8192], F32)
        gg = nc.dram_tensor("gg", [1024, 256], F32)
        gga = nc.dram_tensor("gga", [1024, 256], F32)
        hsd = nc.dram_tensor("hs", [1024, D], F32)
        pbuf = nc.dram_tensor("pbuf", [8192, D], F32)
        pall = nc.dram_tensor("pall", [8192, D], F32)

    C_BQ, C_BK, C_SQ, C_SQR, C_SK, C_SKR, C_GA, C_GB, C_BV = 0, 8, 16, 24, 32, 34, 36, 52, 68

    es = ExitStack()
    with es:
        def sb(name, shape, dt):
            return es.enter_context(nc.sbuf_tensor(name, list(shape), dt))

        def psum(name):
            return es.enter_context(nc.psum_tensor(name, [128, 512], F32))

        XA = sb("XA", [128, 16, 1024], BF16)
        XB = sb("XB", [128, 16, 1024], BF16)
        RH = sb("RH", [128, 8, 2048], F32)
        WBt = [sb("WB0", [128, 8192], BF16), sb("WB1", [128, 8192], BF16)]
        gt1b = sb("gt1b", [128, D], BF16)
        gt2b = sb("gt2b", [128, D], BF16)
        tab = sb("tab", [128, 4096], BF16)
        scr0 = sb("scr0", [128, D], BF16)
        pool4 = sb("pool4", [128, D], BF16)
        biasq = [sb("biasq%d" % i, [128, 16, 16], F32) for i in range(2)]
        bdn = sb("bdn", [128, D], BF16)
        GT = sb("GT", [128, 1024], BF16)
        ident = sb("ident", [128, 128], BF16)
        identf = sb("identf", [128, 128], F32)
        ones_f = sb("ones_f", [128, 128], F32)
        U_f = sb("U_f", [128, 128], F32)
        E64 = sb("E64", [128, 128], F32)
        maskC = sb("maskC", [128, 512], BF16)
        maskP = sb("maskP", [128, 512], BF16)
        zmask = sb("zmask", [128, 128], BF16)
        bcol = sb("bcol", [128, 80], F32)
        bgu = sb("bgu", [128, 4 * 32], F32)
        modcol = sb("modcol", [128, 96], F32)
        oh8 = sb("oh8_sb", [128, 8], F32)
        ohb = sb("ohb_sb", [128, 4], F32)
        cact4 = sb("cact4", [128, 64], F32)
        Gsel = sb("Gsel", [128, 8, 4], F32)
        g84 = sb("g84", [128, 8, 8, 4], F32)
        gs1 = sb("gs1", [128, 16], F32)
        gs2 = sb("gs2", [128, 16], F32)
        stat = sb("stat", [128, 64], F32)
        fakec = sb("fakec", [128, 1], F32)
        zcol = sb("zcol", [128, 1], F32)
        smallf = sb("smallf", [128, 64], F32)
        ipi = sb("ipi", [128, 4], I32)
        wf_t = sb("wf_t", [128, 16, 16], BF16)
        wr_t = sb("wr_t", [128, 16, NE], BF16)
        bsvb = sb("bsvb", [128, 256], F32)
        bfb = sb("bfb", [128, 16], F32)
        brb = sb("brb", [128, NE], F32)
        esink = sb("esink", [128, 16], F32)
        lf = sb("lf", [128, 16, 16], F32)
        negcum = sb("negcum", [128, 16, 16], F32)
        Cbn = sb("Cbn", [128, 8, 16], F32)
        G = sb("G", [128, 8, NE], F32)
        gb16 = sb("gb16", [128, NE], BF16)
        rt0 = sb("rt0", [128, NE], F32)
        rt1 = sb("rt1", [128, NE], F32)
        lg = sb("lg", [128, NE], F32)
        PS = [psum("ps%d" % i) for i in range(8)]

        S = Sched(nc)
        op = S.op

        WB = [t[:] for t in WBt]
        WBf = [t[:].bitcast(F32) for t in WBt]
        RHb = RH[:].rearrange("p a b -> p (a b)").bitcast(BF16)
        yaT = RHb[:, 0:8192].rearrange("p (c t) -> p c t", c=8)
        ybT = RHb[:, 8192:16384].rearrange("p (c t) -> p c t", c=8)
        WS = RHb[:, 16384:32768]
        XBf = XB[:].rearrange("p a b -> p (a b)").bitcast(F32)
        tabf = tab[:].bitcast(F32)
        cosT = tab[:, 0:2048]
        sinT = tab[:, 2048:4096]
        TABN = ["tab0", "tab1", "tab2", "tab3"]
        scr0f = scr0[:].bitcast(F32)
        TF = [(scr0f[:, 0:512], "scr0a"), (scr0f[:, 512:1024], "scr0b"),
              (tabf[:, 0:512], "tab0"), (tabf[:, 512:1024], "tab1"),
              (tabf[:, 1024:1536], "tab2"), (tabf[:, 1536:2048], "tab3")]
        SCR = [(scr0[:], ["scr0a", "scr0b"]), (pool4[:], ["sp0", "sp1", "sp2", "sp3a", "sp3b"])]
        PT = [(pool4[:, i * 512:(i + 1) * 512].rearrange("p (h q) -> p h q", h=4), "sp%d" % i) for i in range(3)]
        ytok = [(pool4[:, 1536:1792], "sp3a"), (pool4[:, 1792:2048], "sp3b")]
        TB = [(pool4[:, 0:512], ["sp0"]), (pool4[:, 512:1024], ["sp1"]), (pool4[:, 1024:1536], ["sp2"]),
              (pool4[:, 1536:2048], ["sp3a", "sp3b"])]
        brow = [(lf[0:1, :, :].rearrange("p a b -> p (a b)"), "lf"), (negcum[0:1, :, :].rearrange("p a b -> p (a b)"), "negcum")]
        iof = WBf[0][:, 0:512]
        W1T = [WBf[1][:, i * 512:(i + 1) * 512] for i in range(4)]
        posi = WBt[1][:].bitcast(I32)[:, 2048:2560]
        PSb = [p[:].bitcast(BF16) for p in PS]
        HN = ["h%d" % i for i in range(8)]

        def mm(reads, writes, lst):
            def fn(e, lst=lst):
                ins = None
                for (o, l, r, st, sp) in lst:
                    ins = e.matmul(o, lhsT=l, rhs=r, start=st, stop=sp)
                return ins
            return op("pe", fn, reads=reads, writes=writes)

        def dma(eng, dst, src, reads, writes):
            return op(eng, lambda e: e.dma_start(out=dst, in_=src), reads=reads, writes=writes, dma=True)

        def act(o, i, func, reads, writes, bias=None, scale=None, accum=None):
            kw = {}
            if bias is not None:
                kw["bias"] = bias
            if scale is not None:
                kw["scale"] = scale
            if accum is not None:
                kw["accum_out"] = accum
            return op("act", lambda e: e.activation(out=o, in_=i, func=func, **kw), reads=reads, writes=writes)

        def ts(o, i, s1, s2, op0, op1, reads, writes, eng="dve"):
            if op1 is None:
                return op(eng, lambda e: e.tensor_scalar(out=o, in0=i, scalar1=s1, scalar2=None, op0=op0),
                          reads=reads, writes=writes)
            return op(eng, lambda e: e.tensor_scalar(out=o, in0=i, scalar1=s1, scalar2=s2, op0=op0, op1=op1),
                      reads=reads, writes=writes)

        def tt(o, a, b, aop, reads, writes, eng="dve"):
            return op(eng, lambda e: e.tensor_tensor(out=o, in0=a, in1=b, op=aop), reads=reads, writes=writes)

        def stt(o, a, s, b, op0, op1, reads, writes, eng="dve"):
            return op(eng, lambda e: e.scalar_tensor_tensor(out=o, in0=a, scalar=s, in1=b, op0=op0, op1=op1),
                      reads=reads, writes=writes)

        def cp(o, i, reads, writes, eng="dve"):
            return op(eng, lambda e: e.tensor_copy(out=o, in_=i), reads=reads, writes=writes)

        def allreduce8(a_, b_, a_res, b_res):
            g1 = [[0, 1, 2, 3], [4, 5, 6, 7]]
            g2 = [[0, 4], [1, 5], [2, 6], [3, 7]]
            op("pool", lambda e: e.collective_compute("AllReduce", ALU.add, replica_groups=g1,
                                                      ins=[a_.opt()], outs=[b_.opt()]),
               reads=a_res, writes=[b_res], cc=True)
            op("pool", lambda e: e.collective_compute("AllReduce", ALU.add, replica_groups=g2,
                                                      ins=[b_.opt()], outs=[a_.opt()]),
               reads=[b_res], writes=a_res, cc=True)

        op("dve", lambda e: e.memset(stat[:], 0.0), writes=["stat"])
        op("pool", lambda e: e.iota(iof.rearrange("p (a b) -> p a b", a=4), pattern=[[0, 4], [1, 128]], base=0,
                                     channel_multiplier=-1, allow_small_or_imprecise_dtypes=True), writes=["WB0"])
        ts(identf[:], iof[:, 0:128], 0.0, None, ALU.is_equal, None, ["WB0"], ["identf"])
        cp(ident[:], identf[:], ["identf"], ["ident"])
        ts(U_f[:], iof[:, 0:128], 0.0, None, ALU.is_ge, None, ["WB0"], ["U_f"])
        ts(maskC[:], iof, 0.0, NEG, ALU.is_lt, ALU.mult, ["WB0"], ["maskC"])
        ts(maskP[:], iof, 0.0, NEG, ALU.is_ge, ALU.mult, ["WB0"], ["maskP"])
        op("dve", lambda e: e.memset(ones_f[:], 1.0), writes=["ones_f"])
        op("dve", lambda e: e.memset(zcol[:], 0.0), writes=["zcol"])
        op("dve", lambda e: e.memset(zmask[:], 0.0), writes=["zmask"])
        op("dve", lambda e: e.memset(GT[:], 0.0), writes=["GT"])
        op("dve", lambda e: e.memset(bdn[:], 0.0), writes=["bdn"])
        op("pool", lambda e: e.iota(E64[:], pattern=[[0, 128]], base=-64, channel_multiplier=1,
                                     allow_small_or_imprecise_dtypes=True), writes=["E64"])
        ts(E64[:], E64[:], 0.0, None, ALU.is_equal, None, ["E64"], ["E64"])
        dma("sp", fakec[:], fake, [], ["fakec"])
        dma("sp", bcol[:], bcols, [], ["bcol"])
        dma("sp", bsvb[:], b_in[0:1, 1280:1536].partition_broadcast(128), [], ["bsvb"])
        dma("sp", bfb[:], b_in[0:1, FG:FG + 16].partition_broadcast(128), [], ["bfb"])
        dma("sp", brb[:], b_r.partition_broadcast(128), [], ["brb"])
        dma("sp", esink[:], sinks.partition_broadcast(128), [], ["esink"])
        act(esink[:], esink[:], AF.Exp, ["esink"], ["esink"])
        dma("sp", oh8[:], oh8d, [], ["oh8"])
        dma("sp", ohb[:], ohbd, [], ["ohb"])
        dma("sp", cact4[:], cT4, [], ["cact4"])
        act(cact4[:], cact4[:], AF.Silu, ["cact4"], ["cact4"])
        dma("sp", smallf[:, 16:32], gmT, [], ["gmTl"])
        dma("sp", smallf[:, 32:48], gfT, [], ["gfTl"])

        op("pool", lambda e: e.iota(ipi[:, 0:1], pattern=[[0, 1]], base=0, channel_multiplier=1), writes=["ipi"])
        op("dve", lambda e: e.tensor_single_scalar(out=ipi[:, 1:2], in_=ipi[:, 0:1], scalar=31, op=ALU.bitwise_and),
           reads=["ipi"], writes=["ipi1"])
        op("dve", lambda e: e.tensor_single_scalar(out=ipi[:, 2:3], in_=ipi[:, 0:1], scalar=32, op=ALU.bitwise_and),
           reads=["ipi"], writes=["ipi2"])
        cp(smallf[:, 48:50], ipi[:, 1:3], ["ipi1", "ipi2"], ["ipf"])
        act(smallf[:, 50:51], smallf[:, 48:49], AF.Exp, ["ipf"], ["invf"], scale=-float(np.log(10000.0)) / 32.0)
        ts(smallf[:, 50:51], smallf[:, 50:51], float(1.0 / (2 * np.pi)), None, ALU.mult, None, ["invf"], ["invf"])
        ts(smallf[:, 51:52], smallf[:, 49:50], 1.0 / 16.0, -1.0, ALU.mult, ALU.add, ["ipf"], ["sgn"])
        for q4 in range(4):
            tsl = slice(q4 * 512, (q4 + 1) * 512)
            dma("sp", posi, posb[0:1, tsl].partition_broadcast(128), [], ["WB1"])
            cp(W1T[0], posi, ["WB1"], ["WB1"])
            ts(W1T[1], W1T[0], smallf[:, 50:51], None, ALU.mult, None, ["WB1", "invf"], ["WB1"])
            for which, dst, off in (("s", sinT, 0.0), ("c", cosT, 0.25)):
                ts(W1T[2], W1T[1], off, None, ALU.add, None, ["WB1"], ["WB1"])
                cp(posi, W1T[2], ["WB1"], ["WB1"])
                cp(W1T[3], posi, ["WB1"], ["WB1"])
                tt(W1T[2], W1T[2], W1T[3], ALU.subtract, ["WB1"], ["WB1"])
                act(W1T[3], W1T[2], AF.Sin, ["WB1"], ["WB1"], scale=float(2 * np.pi))
                if which == "s":
                    ts(dst[:, tsl], W1T[3], smallf[:, 51:52], None, ALU.mult, None, ["WB1", "sgn"], TABN)
                else:
                    cp(dst[:, tsl], W1T[3], ["WB1"], TABN)

        RHf = RH[:].rearrange("p a b -> p (a b)")
        RHN = ["yaT", "ybT", "WS"]
        c4v = cact4[:].rearrange("p (k b) -> p k b", b=4)
        w_ada_v = w_ada_s.rearrange("(k p) c -> p k c", p=128)
        mods = RHf[0:4, 0:1536]
        for j in range(6):
            b = j % 2
            wv = WBf[b].rearrange("p (k c) -> p k c", k=16)
            br_ap, br_n = brow[b]
            dma("sp", wv, w_ada_v[:, :, j * 256:(j + 1) * 256], [], ["WB%d" % b])
            dma("sp", br_ap, b_ada_s[0:1, j * 256:(j + 1) * 256], [], [br_n])
            pr = PS[1 + b]
            prn = "ps%d" % (1 + b)
            lst = [(pr[0:4, 0:256], c4v[:, k, :], wv[:, k, :], k == 0, False) for k in range(16)]
            lst.append((pr[0:4, 0:256], ones_f[0:1, 0:4], br_ap[0:1, :], False, True))
            mm(["WB%d" % b, br_n, "cact4", "ones_f"], [prn], lst)
            cp(mods[:, j * 256:(j + 1) * 256], pr[0:4, 0:256], [prn], RHN)
        expd = RHf[0:4, 2048:2048 + 12288].rearrange("p (s c) -> p s c", s=8)
        tt(expd, mods.unsqueeze(1).broadcast_to([4, 8, 1536]), oh8[0:4, :].unsqueeze(2).broadcast_to([4, 8, 1536]),
           ALU.mult, RHN + ["oh8"], RHN)
        dma("sp", modg, RHf[0:4, 2048:2048 + 12288], RHN, ["modg"])
        if not NOCC:
            allreduce8(modg, moda, ["modg"], "moda")
        mod_src, mod_res = (moda, []) if NOCC else (modg, ["modg"])
        mrow = RHf[0:96, 0:512].rearrange("p (b q) -> p b q", b=4)
        dma("sp", mrow, mod_src.rearrange("b (j q) -> j b q", q=128), mod_res, RHN)
        mmine = RHf[0:96, 512:640]
        ts(mmine, mrow[:, 0, :], ohb[0:96, 0:1], None, ALU.mult, None, RHN + ["ohb"], RHN)
        for bb in range(1, 4):
            stt(mmine, mrow[:, bb, :], ohb[0:96, bb:bb + 1], mmine, ALU.mult, ALU.add, RHN + ["ohb"], RHN)
        op("pe", lambda e: e.transpose(out=PS[0][:, 0:96], in_=mmine, identity=identf[0:96, 0:96]),
           reads=RHN + ["identf"], writes=["ps0"])
        cp(modcol[:], PS[0][:, 0:96], ["ps0"], ["modcol"])
        pidx = RHf[0:96, 640:768]
        op("pool", lambda e: e.iota(pidx, pattern=[[0, 128]], base=0, channel_multiplier=1,
                                     allow_small_or_imprecise_dtypes=True), reads=[], writes=RHN)
        for jj in range(32):
            j0 = (32 + jj) if jj < 16 else (80 + jj - 16)
            sel = RHf[0:96, 768 + (jj % 2) * 128:896 + (jj % 2) * 128]
            ts(sel, pidx, float(j0), None, ALU.is_equal, None, RHN, RHN)
            bk = 1 + (jj % 2)
            mm(RHN, ["ps%d" % bk], [(PS[bk][:, 0:128], sel, mmine, True, True)])
            dstt = (gt1b if jj < 16 else gt2b)[:, (jj % 16) * 128:(jj % 16 + 1) * 128]
            act(dstt, PS[bk][:, 0:128], AF.Copy, ["ps%d" % bk], ["gt1b" if jj < 16 else "gt2b"])
        SH1, SC1, SH2, SC2 = modcol[:, 0:16], modcol[:, 16:32], modcol[:, 48:64], modcol[:, 64:80]
        stt(gs1[:], SC1, 1.0, smallf[:, 16:32], ALU.add, ALU.mult, ["modcol", "gmTl"], ["gs1"])
        stt(gs2[:], SC2, 1.0, smallf[:, 32:48], ALU.add, ALU.mult, ["modcol", "gfTl"], ["gs2"])

        ev_toggle = [0]

        def norm_block(src_ap, src_res, bi, dstX, dst_res, idx, gs, sh_i, statcol):
            sc, scn = SCR[bi % 2]
            stn = "st%d" % statcol
            ssq = stat[:, statcol:statcol + 1]
            act(sc, src_ap, AF.Square, [src_res, "stat"], scn + [stn], accum=ssq)
            ts(ssq, ssq, 1.0 / D, 1e-5, ALU.mult, ALU.add, [stn], [stn])
            act(ssq, ssq, AF.Sqrt, [stn], [stn])
            op("dve", lambda e: e.reciprocal(out=ssq, in_=ssq), reads=[stn], writes=[stn])
            ts(sc, src_ap, ssq, None, ALU.mult, None, [src_res, stn], scn)
            banks = (2, 3) if bi % 2 == 0 else (4, 5)
            for hb in range(2):
                bk = banks[hb]

                def fn(e, bk=bk, hb=hb):
                    ins = None
                    for kk in range(8):
                        k = hb * 8 + kk
                        ins = e.transpose(out=PSb[bk][:, kk * 128:(kk + 1) * 128], in_=sc[:, k * 128:(k + 1) * 128],
                                          identity=ident[:])
                    return ins
                op("pe", fn, reads=scn + ["ident"], writes=["ps%d" % bk])
                for kk in range(8):
                    k = hb * 8 + kk
                    d = dstX[:, k, idx * 128:(idx + 1) * 128]
                    s_ = PSb[bk][:, kk * 128:(kk + 1) * 128]
                    ev_toggle[0] ^= 1
                    if ev_toggle[0]:
                        act(d, s_, AF.Identity, ["ps%d" % bk, "gs1", "gs2", "modcol"], [dst_res],
                            bias=sh_i[:, k:k + 1], scale=gs[:, k:k + 1])
                    else:
                        ts(d, s_, gs[:, k:k + 1], sh_i[:, k:k + 1], ALU.mult, ALU.add,
                           ["ps%d" % bk, "gs1", "gs2", "modcol"], [dst_res])

        for bi in range(16):
            own = bi % 2 == 0
            idx = bi // 2
            src = (xw_own if own else xw_oth)[idx * 128:(idx + 1) * 128, :]
            b = bi % 2
            xb = WBf[b][:, 0:2048]
            dma("sp", xb, src, [], ["WB%d" % b])
            norm_block(xb, "WB%d" % b, bi, XA if own else XB, "XA" if own else "XB", idx, gs1, SH1, bi)

        def Xtok(kidx):
            X = XA if kidx < 8 else XB
            i = kidx % 8
            return X, ("XA" if kidx < 8 else "XB"), slice(i * 128, (i + 1) * 128)

        def kidx_of_slot(s):
            return (s // 2) if s % 2 == 1 else 8 + s // 2

        proj_rr = [0]

        def proj_bank():
            proj_rr[0] = (proj_rr[0] + 1) % 4
            return proj_rr[0]

        def load_w(b, views):
            for d_, s_ in views:
                dma("pool", d_, s_, [], ["WB%d" % b])

        w_in_v = w_in.rearrange("(k p) c -> p k c", p=128)

        if STAGE >= 2:
            dma("pool", wf_t[:], w_in_v[:, :, FG:FG + 16], [], ["wf_t"])
            pfb = PS[6][:, 0:256].rearrange("p (s h) -> p s h", s=16)
            for kidx in range(16):
                X, xn_, tsl = Xtok(kidx)
                mm([xn_, "wf_t"], ["ps6"], [(pfb[:, kidx, :], X[:, k, tsl], wf_t[:, k, :], k == 0, k == 15) for k in range(16)])
            tt(lf[:], pfb, bfb[:, :].unsqueeze(1).broadcast_to([128, 16, 16]), ALU.add, ["ps6", "bfb"], ["lf"])
            act(lf[:], lf[:], AF.Sigmoid, ["lf"], ["lf"])
            act(lf[:], lf[:], AF.Ln, ["lf"], ["lf"])
            pcm = PS[7][:, 0:256].rearrange("p (s h) -> p s h", s=16)
            lst = []
            for s in range(16):
                ks = kidx_of_slot(s)
                for m_ in range(s):
                    lst.append((pcm[:, ks, :], ones_f[:], lf[:, kidx_of_slot(m_), :], m_ == 0, False))
                lst.append((pcm[:, ks, :], U_f[:], lf[:, ks, :], s == 0, True))
            mm(["lf", "ones_f", "U_f"], ["ps7"], lst)
            ts(negcum[:], pcm, -1.0, None, ALU.mult, None, ["ps7"], ["negcum"])
            pcb = PS[0][:, 0:128].rearrange("p (i h) -> p i h", i=8)
            mm(["negcum", "E64"], ["ps0"], [(pcb[:, i, :], E64[:], negcum[:, i, :], True, True) for i in range(8)])
            cp(Cbn[:], pcb, ["ps0"], ["Cbn"])
            ts(negcum[:, 8, :], negcum[:, 8, :], fakec[:, 0:1], None, ALU.add, None, ["negcum", "fakec"], ["negcum"])

        att_rr = [0]
        acc_rr = [0]
        pt_rr = [0]
        yt_rr = [0]

        ACCB = [6, 7, 2, 3]
        tr_rr = [0]

        def attention_unit(i, units, KT, QT_of, V, vres, kres, qres, bias_of, bias_res, mask_of, nq, sink_cols, yT, yres,
                           chunk0, ybias):
            accs = [PS[ACCB[h]][:, 0:65] for h in range(4)]
            accn = ["ps%d" % ACCB[h] for h in range(4)]
            nj = len(units)
            for ji, j in enumerate(units):
                att_rr[0] ^= 1
                sbk = 4 + att_rr[0]
                psv = PS[sbk][:].rearrange("p (h q) -> p h q", h=4)
                lst = []
                if nq == 4:
                    for h4 in range(4):
                        mk = mask_of(j)
                        mk_ap = mk[:, 0:128] if mk is not None else zmask[:]
                        lst.append((psv[:, h4, :], ident[:], mk_ap, True, False))
                        lst.append((psv[:, h4, :], KT(j, h4), QT_of(h4), False, True))
                else:
                    mk = mask_of(j)
                    lst.append((PS[sbk][:], ident[:], mk[:], True, False))
                    lst.append((psv, KT(j, 0), QT_of(0), False, True))
                mm([kres, qres, "ident", "maskC", "maskP", "zmask"], ["ps%d" % sbk], lst)
                pt_rr[0] = (pt_rr[0] + 1) % 3
                pt, ptn = PT[pt_rr[0]]
                if nq == 4:
                    for h4 in range(4):
                        act(pt[:, h4, :], psv[:, h4, :], AF.Exp, ["ps%d" % sbk] + bias_res, [ptn],
                            bias=bias_of(j, h4), scale=0.125)
                else:
                    act(pt, psv, AF.Exp, ["ps%d" % sbk] + bias_res, [ptn], bias=bias_of(j, 0), scale=0.125)
                mm([ptn, vres], accn,
                   [(accs[h4], pt[:, h4, :], V(j, h4), ji == 0, ji == nj - 1) for h4 in range(4)])
            den = smallf[:, 56:60]
            for h4 in range(4):
                if sink_cols is not None:
                    tt(den[:, h4:h4 + 1], accs[h4][:, 64:65], sink_cols[:, h4:h4 + 1], ALU.add, [accn[h4], "esink"], ["den"])
                else:
                    cp(den[:, h4:h4 + 1], accs[h4][:, 64:65], [accn[h4]], ["den"])
            op("dve", lambda e: e.reciprocal(out=den, in_=den), reads=["den"], writes=["den"])
            yt_rr[0] ^= 1
            yt, ytn = ytok[yt_rr[0]]
            for h4 in range(4):
                ts(yt[:, h4 * 64:(h4 + 1) * 64], accs[h4][:, 0:64], den[:, h4:h4 + 1], None, ALU.mult, None,
                   [accn[h4], "den"], [ytn])
            tr_rr[0] ^= 1
            tb_ = tr_rr[0]

            def fn(e):
                ins = None
                for c2 in range(2):
                    ins = e.transpose(out=PSb[tb_][:, c2 * 128:(c2 + 1) * 128], in_=yt[:, c2 * 128:(c2 + 1) * 128],
                                      identity=ident[:])
                return ins
            op("pe", fn, reads=[ytn, "ident"], writes=["ps%d" % tb_])
            if ybias is None:
                act(yT[:, chunk0:chunk0 + 2, i * 128:(i + 1) * 128],
                    PSb[tb_][:, 0:256].rearrange("p (c q) -> p c q", c=2), AF.Copy, ["ps%d" % tb_], [yres])
            else:
                for c2 in range(2):
                    act(yT[:, chunk0 + c2, i * 128:(i + 1) * 128], PSb[tb_][:, c2 * 128:(c2 + 1) * 128], AF.Identity,
                        ["ps%d" % tb_, "bcol"], [yres], bias=bcol[:, ybias + c2:ybias + c2 + 1])

        if STAGE >= 3:
            KTs = WS[:, 0:4096].rearrange("p (c t) -> p c t", c=2)
            QTs = WS[:, 4096:12288].rearrange("p (c t) -> p c t", c=8)
            Vs = RHb[:, 8192:8192 + 4160].rearrange("p (s h c) -> p s h c", s=16, h=4)
            op("dve", lambda e: e.memset(Vs[:, :, :, 64:65], 1.0), writes=["ybT"])
            w_sq_v = w_sq.rearrange("(k p) c -> p k c", p=128)
            w_sqr_v = w_sqr.rearrange("(k p) c -> p k c", p=128)
            w_skr_v = w_skr.rearrange("(k p) c -> p k c", p=128)

            def rope_evac(bkA, bkB, colA, colB, tabsl, dst, dres):
                (ta, tan), (tb2, tbn) = TF[0], TF[1]
                stt(ta, PS[bkA][:], bcol[:, colA:colA + 1], cosT[:, tabsl], ALU.add, ALU.mult,
                    ["ps%d" % bkA, "bcol"] + TABN, [tan])
                stt(tb2, PS[bkB][:], bcol[:, colB:colB + 1], sinT[:, tabsl], ALU.add, ALU.mult,
                    ["ps%d" % bkB, "bcol"] + TABN, [tbn])
                tt(dst, ta, tb2, ALU.add, [tan, tbn], [dres])

            wv = WB[0].rearrange("p (k c) -> p k c", k=16)
            load_w(0, [(wv[:, :, 0:256], w_in_v[:, :, 1024:1280]), (wv[:, :, 256:512], w_skr_v[:, :, :])])
            for kp in range(2):
                for t4 in range(4):
                    X = XA if t4 < 2 else XB
                    xr = "XA" if t4 < 2 else "XB"
                    tsl = slice((t4 % 2) * 512, (t4 % 2 + 1) * 512)
                    gsl = slice(t4 * 512, (t4 + 1) * 512)
                    bA, bB = proj_bank(), proj_bank()
                    mm(["WB0", xr], ["ps%d" % bA], [(PS[bA][:], wv[:, k, kp * 128:(kp + 1) * 128], X[:, k, tsl], k == 0, k == 15) for k in range(16)])
                    mm(["WB0", xr], ["ps%d" % bB], [(PS[bB][:], wv[:, k, 256 + kp * 128:256 + (kp + 1) * 128], X[:, k, tsl], k == 0, k == 15) for k in range(16)])
                    rope_evac(bA, bB, C_SK + kp, C_SKR + kp, gsl, KTs[:, kp, gsl], "WS")
            wv1 = WB[1].rearrange("p (k c) -> p k c", k=16)
            load_w(1, [(wv1[:, :, 0:256], w_in_v[:, :, 1280:1536])])
            for kidx in range(16):
                X, xr, tsl = Xtok(kidx)
                bk = proj_bank()
                mm(["WB1", xr], ["ps%d" % bk], [(PS[bk][:, 0:256], X[:, k, tsl], wv1[:, k, 0:256], k == 0, k == 15) for k in range(16)])
                tt(Vs[:, kidx, :, 0:64], PS[bk][:, 0:256].rearrange("p (h c) -> p h c", h=4),
                   bsvb[:].rearrange("p (h c) -> p h c", h=4), ALU.add, ["ps%d" % bk, "bsvb"], ["ybT"])
            for m4 in range(4):
                b = m4 % 2
                wq = WB[b].rearrange("p (k c) -> p k c", k=16)
                load_w(b, [(wq[:, :, 0:256], w_sq_v[:, :, m4 * 256:(m4 + 1) * 256]),
                           (wq[:, :, 256:512], w_sqr_v[:, :, m4 * 256:(m4 + 1) * 256])])
                for c2 in range(2):
                    c = m4 * 2 + c2
                    for t2 in range(2):
                        tsl = slice(t2 * 512, (t2 + 1) * 512)
                        bA, bB = proj_bank(), proj_bank()
                        mm(["WB%d" % b, "XA"], ["ps%d" % bA], [(PS[bA][:], wq[:, k, c2 * 128:(c2 + 1) * 128], XA[:, k, tsl], k == 0, k == 15) for k in range(16)])
                        mm(["WB%d" % b, "XA"], ["ps%d" % bB], [(PS[bB][:], wq[:, k, 256 + c2 * 128:256 + (c2 + 1) * 128], XA[:, k, tsl], k == 0, k == 15) for k in range(16)])
                        rope_evac(bA, bB, C_SQ + c, C_SQR + c, tsl, QTs[:, c, tsl], "WS")
            for i in range(8):
                for kvh in range(4):
                    hh, kp = kvh % 2, kvh // 2
                    hs = slice(hh * 64, (hh + 1) * 64)
                    attention_unit(
                        i, [8 + i, i],
                        KT=lambda j, h4, hs=hs, kp=kp: KTs[hs, kp, j * 128:(j + 1) * 128],
                        QT_of=lambda h4, hs=hs, kp=kp, i=i: QTs[hs, kp * 4:(kp + 1) * 4, i * 128:(i + 1) * 128],
                        V=lambda j, h4, kvh=kvh: Vs[:, j, kvh, :],
                        vres="ybT", kres="WS", qres="WS",
                        bias_of=lambda j, h4, i=i: (fakec[:, 0:1] if (i == 0 and j == 8) else zcol[:, 0:1]),
                        bias_res=["fakec", "zcol"],
                        mask_of=lambda j: (maskP if j >= 8 else maskC),
                        nq=1, sink_cols=esink[:, 4 * kvh:4 * kvh + 4], yT=yaT, yres="yaT", chunk0=2 * kvh, ybias=None)

        if STAGE >= 4:
            KTg = WS[:, 0:4096].rearrange("p (c t) -> p c t", c=2)
            QTg = WS[:, 4096:6144].rearrange("p (c t) -> p c t", c=2)
            Vg = WS[:, 6144:6144 + 4160].rearrange("p (s h c) -> p s h c", s=16, h=4)
            for g in range(int(os.environ.get('MK_FOXG', '4'))):
                wqk = WB[0].rearrange("p (k c) -> p k c", k=16)
                load_w(0, [(wqk[:, :, 0:256], w_in_v[:, :, 1536 + 256 * g:1536 + 256 * (g + 1)]),
                           (wqk[:, :, 256:512], w_in_v[:, :, 2560 + 256 * g:2560 + 256 * (g + 1)])])
                wv1 = WB[1].rearrange("p (k c) -> p k c", k=16)
                load_w(1, [(wv1[:, :, 0:256], w_in_v[:, :, 3584 + 256 * g:3584 + 256 * (g + 1)])])
                for pr in range(2):
                    for t4 in range(4):
                        X = XA if t4 < 2 else XB
                        xr = "XA" if t4 < 2 else "XB"
                        tsl = slice((t4 % 2) * 512, (t4 % 2 + 1) * 512)
                        gsl = slice(t4 * 512, (t4 + 1) * 512)
                        bk = proj_bank()
                        mm(["WB0", xr], ["ps%d" % bk], [(PS[bk][:], wqk[:, k, 256 + pr * 128:256 + (pr + 1) * 128], X[:, k, tsl], k == 0, k == 15) for k in range(16)])
                        act(KTg[:, pr, gsl], PS[bk][:], AF.Identity, ["ps%d" % bk, "bcol"], ["WS"],
                            bias=bcol[:, C_BK + 2 * g + pr:C_BK + 2 * g + pr + 1])
                    for t2 in range(2):
                        tsl = slice(t2 * 512, (t2 + 1) * 512)
                        bk = proj_bank()
                        mm(["WB0", "XA"], ["ps%d" % bk], [(PS[bk][:], wqk[:, k, pr * 128:(pr + 1) * 128], XA[:, k, tsl], k == 0, k == 15) for k in range(16)])
                        act(QTg[:, pr, tsl], PS[bk][:], AF.Identity, ["ps%d" % bk, "bcol"], ["WS"],
                            bias=bcol[:, C_BQ + 2 * g + pr:C_BQ + 2 * g + pr + 1])
                op("dve", lambda e: e.memset(Vg[:, :, :, 64:65], 1.0), writes=["WS"])
                for kidx in range(16):
                    X, xr, tsl = Xtok(kidx)
                    bk = proj_bank()
                    mm(["WB1", xr], ["ps%d" % bk], [(PS[bk][:, 0:256], X[:, k, tsl], wv1[:, k, 0:256], k == 0, k == 15) for k in range(16)])
                    cp(Vg[:, kidx, :, 0:64], PS[bk][:, 0:256].rearrange("p (h c) -> p h c", h=4), ["ps%d" % bk], ["WS"])
                for i in range(int(os.environ.get('MK_FOXI', '8'))):
                    bq, bqn = biasq[i % 2], "biasq%d" % (i % 2)
                    tt(bq[:], negcum[:], Cbn[:, i, :].unsqueeze(1).broadcast_to([128, 16, 16]), ALU.subtract,
                       ["negcum", "Cbn"], [bqn])
                    units = [8 + m_ for m_ in range(i + 1)] + list(range(i + 1))
                    attention_unit(
                        i, units,
                        KT=lambda j, h4: KTg[(h4 % 2) * 64:(h4 % 2 + 1) * 64, h4 // 2, j * 128:(j + 1) * 128],
                        QT_of=lambda h4, i=i: QTg[(h4 % 2) * 64:(h4 % 2 + 1) * 64, h4 // 2, i * 128:(i + 1) * 128],
                        V=lambda j, h4: Vg[:, j, h4, :],
                        vres="WS", kres="WS", qres="WS",
                        bias_of=lambda j, h4, bq=bq, g=g: bq[:, j, 4 * g + h4:4 * g + h4 + 1],
                        bias_res=[bqn],
                        mask_of=lambda j, i=i: (maskC if j == i else None),
                        nq=4, sink_cols=None, yT=ybT, yres="ybT", chunk0=2 * g, ybias=C_BV + 2 * g)

        mergedT = XB
        if STAGE >= 5:
            w_ba_v = w_ba.rearrange("(k p) c -> p k c", p=128)
            w_bb_v = w_bb.rearrange("(k p) c -> p k c", p=128)
            for dc in range(16):
                b = dc % 2
                wa = WB[b][:, 0:1024].rearrange("p (k c) -> p k c", k=8)
                wb_ = WB[b][:, 1024:2048].rearrange("p (k c) -> p k c", k=8)
                wga = WB[b][:, 2048:4096].rearrange("p (k c) -> p k c", k=16)
                wgb = WB[b][:, 4096:6144].rearrange("p (k c) -> p k c", k=16)
                cs = slice(dc * 128, (dc + 1) * 128)
                load_w(b, [(wa, w_ba_v[:, :, cs]), (wb_, w_bb_v[:, :, cs]),
                           (wga, w_in_v[:, :, GA0 + dc * 128:GA0 + (dc + 1) * 128]),
                           (wgb, w_in_v[:, :, GB0 + dc * 128:GB0 + (dc + 1) * 128])])
                for t2 in range(2):
                    tsl = slice(t2 * 512, (t2 + 1) * 512)
                    u = (dc * 2 + t2) % 2
                    base = 4 * u
                    bGA, bGB, bA, bB = base, base + 1, base + 2, base + 3
                    wn = "WB%d" % b
                    mm([wn, "XA"], ["ps%d" % bGA], [(PS[bGA][:], wga[:, k, :], XA[:, k, tsl], k == 0, k == 15) for k in range(16)])
                    mm([wn, "XA"], ["ps%d" % bGB], [(PS[bGB][:], wgb[:, k, :], XA[:, k, tsl], k == 0, k == 15) for k in range(16)])
                    mm([wn, "yaT"], ["ps%d" % bA], [(PS[bA][:], wa[:, k, :], yaT[:, k, tsl], k == 0, k == 7) for k in range(8)])
                    mm([wn, "ybT"], ["ps%d" % bB], [(PS[bB][:], wb_[:, k, :], ybT[:, k, tsl], k == 0, k == 7) for k in range(8)])
                    (sA, sAn), (sB, sBn) = TB[2 * u], TB[2 * u + 1]
                    act(sA, PS[bGA][:], AF.Sigmoid, ["ps%d" % bGA, "bcol"], sAn, bias=bcol[:, C_GA + dc:C_GA + dc + 1])
                    act(sB, PS[bGB][:], AF.Sigmoid, ["ps%d" % bGB, "bcol"], sBn, bias=bcol[:, C_GB + dc:C_GB + dc + 1])
                    (t1, t1n), (t2_, t2n) = TF[2 * u], TF[2 * u + 1]
                    tt(t1, PS[bA][:], sA, ALU.mult, ["ps%d" % bA] + sAn, [t1n])
                    tt(t2_, PS[bB][:], sB, ALU.mult, ["ps%d" % bB] + sBn, [t2n])
                    tt(mergedT[:, dc, tsl], t1, t2_, ALU.add, [t1n, t2n], ["XB"])

        if STAGE >= 6:
            for tb in range(8):
                dma("sp", RH[:, tb, :], xw_own[tb * 128:(tb + 1) * 128, :], [], [HN[tb], "yaT", "ybT", "WS"])
            w_out_v = w_out.rearrange("(k p) c -> p k c", p=128)
            for dq in range(4):
                b = dq % 2
                wn = "WB%d" % b
                wv = WB[b].rearrange("p (k c) -> p k c", k=16)
                cs = slice(dq * 512, (dq + 1) * 512)
                load_w(b, [(wv, w_out_v[:, :, cs])])
                tt(wv, wv, gt1b[:, cs].unsqueeze(1).broadcast_to([128, 16, 512]), ALU.mult, [wn, "gt1b"], [wn])
                for tb in range(8):
                    bk = proj_bank()
                    mm([wn, "XB"], ["ps%d" % bk], [(PS[bk][:], mergedT[:, k, tb * 128:(tb + 1) * 128], wv[:, k, :], k == 0, k == 15) for k in range(16)])
                    tt(RH[:, tb, cs], RH[:, tb, cs], PS[bk][:], ALU.add, [HN[tb], "ps%d" % bk], [HN[tb]])

        if STAGE >= 7:
            dma("pool", wr_t[:], w_r.rearrange("(k p) c -> p k c", p=128), [], ["wr_t"])
            dma("pool", bdn[0:NE, :], b_dn, [], ["bdn"])
            tt(bdn[0:NE, :], bdn[0:NE, :], gt2b[0:NE, :], ALU.mult, ["bdn", "gt2b"], ["bdn"])
            for tb in range(8):
                norm_block(RH[:, tb, :], HN[tb], tb, XA, "XA", tb, gs2, SH2, 16 + tb)
                tsl = slice(tb * 128, (tb + 1) * 128)
                bk = proj_bank()
                mm(["XA", "wr_t"], ["ps%d" % bk], [(PS[bk][:, 0:NE], XA[:, k, tsl], wr_t[:, k, :], k == 0, k == 15) for k in range(16)])
                tt(lg[:], PS[bk][:, 0:NE], brb[:], ALU.add, ["ps%d" % bk, "brb"], ["lg"])
                m8 = smallf[:, 0:8]
                op("dve", lambda e: e.max(out=m8, in_=lg[:]), reads=["lg"], writes=["m8"])
                ts(smallf[:, 8:9], smallf[:, 0:1], -1.0, None, ALU.mult, None, ["m8"], ["nmx"])
                act(rt0[:], lg[:], AF.Exp, ["lg", "nmx"], ["rt0"], bias=smallf[:, 8:9])
                ts(rt1[:], lg[:], smallf[:, 3:4], None, ALU.is_ge, None, ["lg", "m8"], ["rt1"])
                tt(rt0[:], rt0[:], rt1[:], ALU.mult, ["rt0", "rt1"], ["rt0"])
                op("dve", lambda e: e.reduce_sum(out=smallf[:, 9:10], in_=rt0[:], axis=AX.X), reads=["rt0"], writes=["gden"])
                op("dve", lambda e: e.reciprocal(out=smallf[:, 9:10], in_=smallf[:, 9:10]), reads=["gden"], writes=["gden"])
                ts(G[:, tb, :], rt0[:], smallf[:, 9:10], None, ALU.mult, None, ["rt0", "gden"], ["G"])
                cp(gb16[:], G[:, tb, :], ["G"], ["gb16"])
                bk2 = proj_bank()
                op("pe", lambda e, bk2=bk2: e.transpose(out=PSb[bk2][0:NE, 0:128], in_=gb16[:], identity=ident[:]),
                   reads=["gb16", "ident"], writes=["ps%d" % bk2])
                act(GT[0:NE, tsl], PSb[bk2][0:NE, 0:128], AF.Copy, ["ps%d" % bk2], ["GT"])
            for tb in range(8):
                for dq in range(4):
                    cs = slice(dq * 512, (dq + 1) * 512)
                    bk = proj_bank()
                    mm(["GT", "bdn"], ["ps%d" % bk], [(PS[bk][:], GT[:, tb * 128:(tb + 1) * 128], bdn[:, cs], True, True)])
                    tt(RH[:, tb, cs], RH[:, tb, cs], PS[bk][:], ALU.add, [HN[tb], "ps%d" % bk], [HN[tb]])

        if STAGE >= 8:
            actT = XB
            XAw = XA[:].rearrange("p a b -> p (a b)").bitcast(F32)
            XBw = XB[:].rearrange("p a b -> p (a b)").bitcast(F32)
            XAflat = XA[:].rearrange("p a b -> p (a b)")
            XBflat = XB[:].rearrange("p a b -> p (a b)")
            dma("sp", bgu[:], b_guT, [], ["bgu"])
            dma("sp", hsd.ap().rearrange("(t p) d -> p t d", p=128), RH[:], HN, ["hs"])
            ggt = tabf[:, 0:2048].rearrange("p (s c) -> p s c", s=8)
            tt(ggt, G[:].rearrange("p a b -> p (a b)").unsqueeze(1).broadcast_to([128, 8, 256]),
               oh8[:].unsqueeze(2).broadcast_to([128, 8, 256]), ALU.mult, ["G", "oh8"], TABN)
            dma("sp", gg.ap().rearrange("(s p) c -> p s c", p=128), ggt, TABN, ["gg"])
            for s8 in range(8):
                ts(XBflat, XAflat, oh8[:, s8:s8 + 1], None, ALU.mult, None, ["XA", "oh8"], ["XB"])
                dma("sp", xg.ap()[s8 * 128:(s8 + 1) * 128, :], XBw, ["XB"], ["xg%d" % s8])
            allreduce8(gg.ap(), gga.ap(), ["gg"], "gga")
            for s8 in range(8):
                allreduce8(xg.ap()[s8 * 128:(s8 + 1) * 128, :], xga.ap()[s8 * 128:(s8 + 1) * 128, :], ["xg%d" % s8], "xga%d" % s8)
            wb_rr = [0]
            up_rr = [0]
            dn_rr = [0]
            n_ch = int(os.environ.get("MK_NCH", "8"))
            for c8 in range(n_ch):
                dma("sp", XAw, xg.ap()[c8 * 128:(c8 + 1) * 128, :], ["xg%d" % c8], ["XA"])
                dma("sp", G[:].rearrange("p a b -> p (a b)"), gg.ap()[c8 * 128:(c8 + 1) * 128, :], ["gg"], ["G"])
                tt(g84[:], G[:].rearrange("p t (r l) -> p t r l", r=8),
                   oh8[:].unsqueeze(1).unsqueeze(3).broadcast_to([128, 8, 8, 4]), ALU.mult, ["G", "oh8"], ["g84"])
                op("dve", lambda e: e.reduce_sum(out=Gsel[:], in_=g84[:].rearrange("p t r l -> p t l r"), axis=AX.X),
                   reads=["g84"], writes=["Gsel"])
                for le in range(4):
                    for p8 in range(8):
                        b = wb_rr[0]
                        wb_rr[0] ^= 1
                        wn = "WB%d" % b
                        wv = WB[b].rearrange("p (k two c) -> p k two c", k=16, two=2)
                        srcv = w_gu[le].rearrange("(k p) c -> p k c", p=128)
                        load_w(b, [(wv[:, :, 0, :], srcv[:, :, p8 * 256:(p8 + 1) * 256]),
                                   (wv[:, :, 1, :], srcv[:, :, 2048 + p8 * 256:2048 + (p8 + 1) * 256])])
                        for fc in range(2):
                            ch = p8 * 2 + fc
                            for t2 in range(2):
                                tsl = slice(t2 * 512, (t2 + 1) * 512)
                                up_rr[0] = (up_rr[0] + 1) % 3
                                bG, bL = 2 * up_rr[0], 2 * up_rr[0] + 1
                                mm([wn, "XA"], ["ps%d" % bG], [(PS[bG][:], wv[:, k, 0, fc * 128:(fc + 1) * 128], XA[:, k, tsl], k == 0, k == 15) for k in range(16)])
                                mm([wn, "XA"], ["ps%d" % bL], [(PS[bL][:], wv[:, k, 1, fc * 128:(fc + 1) * 128], XA[:, k, tsl], k == 0, k == 15) for k in range(16)])
                                u = (ch * 2 + t2) % 2
                                (tg, tgn), (tl, tln), (tsg, tsn) = TF[3 * u], TF[3 * u + 1], TF[3 * u + 2]
                                bgc = bgu[:, le * 32 + ch:le * 32 + ch + 1]
                                blc = bgu[:, le * 32 + 16 + ch:le * 32 + 16 + ch + 1]
                                ts(tg, PS[bG][:], bgc, 7.0, ALU.add, ALU.min, ["ps%d" % bG, "bgu"], [tgn])
                                act(tsg, tg, AF.Sigmoid, [tgn], [tsn], scale=1.702)
                                ts(tl, PS[bL][:], blc, 7.0, ALU.add, ALU.min, ["ps%d" % bL, "bgu"], [tln])
                                ts(tl, tl, -7.0, 1.0, ALU.max, ALU.add, [tln], [tln])
                                tt(tg, tg, tsg, ALU.mult, [tgn, tsn], [tgn])
                                tt(actT[:, ch, tsl], tg, tl, ALU.mult, [tgn, tln], ["XB"])
                    for q4 in range(4):
                        b = wb_rr[0]
                        wb_rr[0] ^= 1
                        wn = "WB%d" % b
                        wv = WB[b].rearrange("p (k c) -> p k c", k=16)
                        cs = slice(q4 * 512, (q4 + 1) * 512)
                        load_w(b, [(wv, w_dn[le].rearrange("(k p) c -> p k c", p=128)[:, :, cs])])
                        for tb in range(8):
                            dn_rr[0] ^= 1
                            bk = 6 + dn_rr[0]
                            mm([wn, "XB"], ["ps%d" % bk], [(PS[bk][:], actT[:, k, tb * 128:(tb + 1) * 128], wv[:, k, :], k == 0, k == 15) for k in range(16)])
                            if le == 0:
                                ts(RH[:, tb, cs], PS[bk][:], Gsel[:, tb, 0:1], None, ALU.mult, None,
                                   ["ps%d" % bk, "Gsel", "hs"], [HN[tb]])
                            else:
                                stt(RH[:, tb, cs], PS[bk][:], Gsel[:, tb, le:le + 1], RH[:, tb, cs], ALU.mult, ALU.add,
                                    ["ps%d" % bk, "Gsel", HN[tb]], [HN[tb]])
                for tb in range(8):
                    dma("sp", pbuf.ap()[c8 * 1024 + tb * 128:c8 * 1024 + (tb + 1) * 128, :], RH[:, tb, :], [HN[tb]],
                        ["pb%d_%d" % (c8, tb)])
                for hf in range(2):
                    r0 = c8 * 1024 + hf * 512
                    allreduce8(pbuf.ap()[r0:r0 + 512, :], pall.ap()[r0:r0 + 512, :],
                               ["pb%d_%d" % (c8, tb) for tb in range(hf * 4, hf * 4 + 4)], "pall%d_%d" % (c8, hf))
            XAq = XAw.rearrange("p (s d) -> p s d", s=4)
            XBq = XBw.rearrange("p (s d) -> p s d", s=4)
            pall_v = pbuf.ap().rearrange("(s t p) d -> t p s d", s=8, p=128)
            macc = tabf[:, 0:2048]
            for tb in range(8):
                PBN = ["pb%d_%d" % (s_, tb) for s_ in range(n_ch)]
                dma("sp", RH[:, tb, :], hsd.ap()[tb * 128:(tb + 1) * 128, :], ["hs"], [HN[tb]])
                dma("sp", XAq, pall_v[tb, :, 0:4, :], PBN, ["XA"])
                dma("sp", XBq, pall_v[tb, :, 4:8, :], PBN, ["XB"])
                ts(macc, XAq[:, 0, :], oh8[:, 0:1], None, ALU.mult, None, ["XA", "oh8"], TABN)
                for s8 in range(1, 8):
                    srcq = XAq[:, s8, :] if s8 < 4 else XBq[:, s8 - 4, :]
                    stt(macc, srcq, oh8[:, s8:s8 + 1], macc, ALU.mult, ALU.add, ["XA", "XB", "oh8"] + TABN, TABN)
                tt(macc, macc, gt2b[:], ALU.mult, TABN + ["gt2b"], TABN)
                tt(RH[:, tb, :], RH[:, tb, :], macc, ALU.add, [HN[tb]] + TABN, [HN[tb]])

        if STAGE >= 6:
            if STAGE >= 9:
                gfb = WBf[0][:, 0:2048]
                dma("sp", gfb, gfin.partition_broadcast(128), [], ["WB0"])
            for tb in range(8):
                if STAGE >= 9:
                    col = 32 + tb
                    stn = "st%d" % col
                    ssq = stat[:, col:col + 1]
                    sc, scn = SCR[tb % 2]
                    act(sc, RH[:, tb, :], AF.Square, [HN[tb], "stat"], scn + [stn], accum=ssq)
                    ts(ssq, ssq, 1.0 / D, 1e-5, ALU.mult, ALU.add, [stn], [stn])
                    act(ssq, ssq, AF.Sqrt, [stn], [stn])
                    op("dve", lambda e, ssq=ssq: e.reciprocal(out=ssq, in_=ssq), reads=[stn], writes=[stn])
                    stt(RH[:, tb, :], RH[:, tb, :], ssq, gfb, ALU.mult, ALU.mult, [HN[tb], stn, "WB0"], [HN[tb]])
                dma("sp", out[tb * 128:(tb + 1) * 128, :], RH[:, tb, :], [HN[tb]], [])
        else:
            dbg = {1: ("XA", XA[:].rearrange("p a b -> p (a b)").bitcast(F32)),
                   2: ("negcum", negcum[:].rearrange("p a b -> p (a b)")),
                   3: ("yaT", RH[:].rearrange("p a b -> p (a b)")[:, 0:4096]),
                   4: ("ybT", RH[:].rearrange("p a b -> p (a b)")[:, 4096:8192]),
                   5: ("XB", XB[:].rearrange("p a b -> p (a b)").bitcast(F32))}[STAGE]
            n = dbg[1].shape[1]
            if n >= 2048:
                dma("sp", out.rearrange("(a p) c -> p a c", p=128)[:, 0:(n // 2048), :],
                    dbg[1].rearrange("p (a c) -> p a c", c=2048), [dbg[0], "gs1", "modcol", "negcum", "lf", "Cbn"], [])
            else:
                dma("sp", out[0:128, 0:n], dbg[1], [dbg[0], "negcum", "lf", "Cbn"], [])

        S.finish("sp", [(("dma", i), S.dma_use[i] * 16) for i in range(S.n_dma) if S.dma_use[i]])
        S.emit(es)
    return nc


_PROGRAM = None


def _prep(inputs):
    f = lambda a: np.ascontiguousarray(a, dtype=np.float32)
    x = f(inputs["x"]); c = f(inputs["c"]); pos = np.ascontiguousarray(inputs["positions"]).astype(np.int32)
    w_in = f(inputs["w_in"][0]); b_in = f(inputs["b_in"][0])
    colT = lambda v: np.ascontiguousarray(v.reshape(-1, 128).T)
    order = []
    for cch in range(8):
        for half in range(2):
            qh = 8 * (cch // 4) + 4 * half + cch % 4
            order.extend(range(qh * 64, (qh + 1) * 64))
    order = np.array(order)
    rot = lambda cols: (cols // 64) * 64 + (cols % 64 + 32) % 64
    w_sq = np.ascontiguousarray(w_in[:, order]); w_sqr = np.ascontiguousarray(w_in[:, rot(order)])
    kcols = 1024 + np.arange(256)
    krot = 1024 + rot(np.arange(256))
    w_skr = np.ascontiguousarray(w_in[:, krot])
    bcols = np.zeros((128, 80), np.float32)
    bcols[:, 0:8] = colT(b_in[0 + 1536:1536 + 1024])
    bcols[:, 8:16] = colT(b_in[2560:2560 + 1024])
    bcols[:, 16:24] = colT(b_in[order]); bcols[:, 24:32] = colT(b_in[rot(order)])
    bcols[:, 32:34] = colT(b_in[kcols]); bcols[:, 34:36] = colT(b_in[krot])
    bcols[:, 36:52] = colT(b_in[GA0:GA0 + 2048]); bcols[:, 52:68] = colT(b_in[GB0:GB0 + 2048])
    bcols[:, 68:76] = colT(b_in[3584:4608])
    b_gu = f(inputs["b_gu"][0])
    w_ada = f(inputs["w_ada"][0]); b_ada = f(inputs["b_ada"][0]).reshape(1, -1)
    w_gu = f(inputs["w_gu"][0]); w_dn = f(inputs["w_down"][0])
    cT4 = np.ascontiguousarray(c.reshape(4, 16, 128).transpose(2, 1, 0).reshape(128, 64))
    shared = {
        "gmT": colT(f(inputs["g_mix"][0])), "gfT": colT(f(inputs["g_ffn"][0])),
        "gfin": f(inputs["g_final"]).reshape(1, -1), "cT4": cT4,
        "w_in": w_in, "b_in": b_in.reshape(1, -1), "w_sq": w_sq, "w_sqr": w_sqr, "w_skr": w_skr,
        "bcols": bcols, "sinks": f(inputs["sinks"][0]).reshape(1, 16),
        "w_ba": f(inputs["w_branch_a"][0]), "w_bb": f(inputs["w_branch_b"][0]), "w_out": f(inputs["w_out"][0]),
        "w_r": f(inputs["w_router"][0]), "b_r": f(inputs["b_router"][0]).reshape(1, -1),
        "b_dn": f(inputs["b_down"][0]),
    }
    maps = []
    for cid in range(8):
        b, p = cid // 2, cid % 2
        if p == 1:
            xw = x[b]; pw = pos[b]
        else:
            xw = np.concatenate([np.zeros((128, D), np.float32), x[b, :1920]], 0)
            pw = np.concatenate([np.zeros((128,), np.int32), pos[b, :1920]], 0)
        xw = xw.reshape(8, 2, 128, D); pw = pw.reshape(8, 2, 128)
        m = dict(shared)
        m["xw_own"] = np.ascontiguousarray(xw[:, 1].reshape(1024, D))
        m["xw_oth"] = np.ascontiguousarray(xw[:, 0].reshape(1024, D))
        m["posb"] = np.ascontiguousarray(np.concatenate([pw[:, 1].reshape(-1), pw[:, 0].reshape(-1)])[None, :])
        m["fake"] = np.full((128, 1), NEG if p == 0 else 0.0, np.float32)
        oh8 = np.zeros((128, 8), np.float32); oh8[:, cid] = 1.0
        ohb = np.zeros((128, 4), np.float32); ohb[:, b] = 1.0
        m["oh8"] = oh8; m["ohb"] = ohb
        if NOCC:
            cf = c.astype(np.float64)
            m["moda_dbg"] = ((cf / (1 + np.exp(-cf))) @ w_ada.astype(np.float64) + b_ada).astype(np.float32)
        m["w_ada_s"] = np.ascontiguousarray(w_ada[:, cid * 1536:(cid + 1) * 1536])
        m["b_ada_s"] = np.ascontiguousarray(b_ada[:, cid * 1536:(cid + 1) * 1536])
        if STAGE >= 8:
            m["w_gu_l"] = w_gu[4 * cid:4 * cid + 4]
            m["w_dn_l"] = w_dn[4 * cid:4 * cid + 4]
            m["b_guT_l"] = np.ascontiguousarray(
                b_gu[4 * cid:4 * cid + 4].reshape(4, 32, 128).transpose(2, 0, 1).reshape(128, 4 * 32))
        maps.append(m)
    return maps


def kernel(**inputs):
    global _PROGRAM
    if _PROGRAM is None:
        _PROGRAM = build_program()
    maps = _prep(inputs)
    res = run_bass_kernel_spmd(_PROGRAM, maps, core_ids=list(range(8)))
    outf = np.zeros((4, 2048, D), np.float32)
    o4 = outf.reshape(4, 8, 2, 128, D)
    for cid in range(8):
        b, p = cid // 2, cid % 2
        o = np.asarray(res.results[cid]["out"]).reshape(8, 128, D)
        o4[b, :, p] = o
    return outf
```

```python
import os
from contextlib import ExitStack
import numpy as np
import concourse.bass as bass
import concourse.mybir as mybir
from concourse.bass_utils import run_bass_kernel_spmd

F32 = mybir.dt.float32
BF16 = mybir.dt.bfloat16
I32 = mybir.dt.int32
AF = mybir.ActivationFunctionType
ALU = mybir.AluOpType
AX = mybir.AxisListType

D = 2048
NE = 32
FG = 1024 + 256 + 256 + 3 * 1024
GA0 = FG + 16
GB0 = GA0 + 2048
NEG = -30000.0
COMPUTE = ("pe", "act", "dve", "pool")
STAGE = int(os.environ.get("MK_STAGE", "99"))
NOCC = os.environ.get("MK_NOCC", "0") == "1"


class Sched:
    def __init__(self, nc, n_dma_sems=32, same_engine_sync=True):
        self.nc = nc
        self.ops = {e: [] for e in ("pe", "act", "dve", "pool", "sp")}
        self.count = {e: 0 for e in COMPUTE}
        self.res = {}
        self.seen = {e: {} for e in self.ops}
        self.n_dma = n_dma_sems
        self.dma_use = [0] * n_dma_sems
        self.dma_next = 0
        self.same = same_engine_sync
        self.sems = {}
        self.final = []

    @staticmethod
    def _need(need, key, val):
        if val > need.get(key, 0):
            need[key] = val

    def op(self, eng, fn, reads=(), writes=(), dma=False, cc=False):
        need = {}
        for r in reads:
            st = self.res.get(r)
            if st and st["w"]:
                self._need(need, *st["w"])
        for r in writes:
            st = self.res.get(r)
            if st:
                if st["w"]:
                    self._need(need, *st["w"])
                for k, v in st["r"].items():
                    self._need(need, k, v)
        if cc:
            self.n_cc = getattr(self, "n_cc", 0) + 1
            me = (("cc", self.n_cc), 1)
        elif dma:
            s = self.dma_next
            self.dma_next = (s + 1) % self.n_dma
            if self.dma_use[s] > 0:
                self._need(need, ("dma", s), self.dma_use[s] * 16)
            self.dma_use[s] += 1
            me = (("dma", s), self.dma_use[s] * 16)
        else:
            self.count[eng] += 1
            me = (eng, self.count[eng])
        waits = []
        for k, v in need.items():
            if k == eng and not dma and not cc:
                if eng == "pe" or not self.same:
                    continue
            if self.seen[eng].get(k, 0) >= v:
                continue
            self.seen[eng][k] = v
            waits.append((k, v))
        self.ops[eng].append((waits, fn, me))
        for r in reads:
            st = self.res.setdefault(r, {"w": None, "r": {}})
            st["r"][me[0]] = max(st["r"].get(me[0], 0), me[1])
        for r in writes:
            self.res[r] = {"w": me, "r": {}}
        return me

    def finish(self, eng, deps):
        self.final.append((eng, list(deps)))

    def emit(self, es):
        nc = self.nc
        keys = list(COMPUTE) + [("dma", i) for i in range(self.n_dma)] + [("cc", i + 1) for i in range(getattr(self, "n_cc", 0))]
        for k in keys:
            nm = "s_" + (k if isinstance(k, str) else "%s%d" % (k[0][0], k[1]))
            self.sems[k] = es.enter_context(nc.semaphore(nm))
        block = es.enter_context(nc.Block())
        sems = self.sems

        def run(engname):
            def body(e):
                for waits, fn, me in self.ops[engname]:
                    for k, v in waits:
                        e.wait_ge(sems[k], v)
                    ins = fn(e)
                    if isinstance(me[0], tuple) and me[0][0] == "cc":
                        ins.then_inc(sems[me[0]])
                    else:
                        ins.then_inc(sems[me[0]], 16 if isinstance(me[0], tuple) else 1)
                for en, deps in self.final:
                    if en == engname:
                        for k, v in deps:
                            e.wait_ge(sems[k], v)
            return body

        block.tensor(run("pe"))
        block.scalar(run("act"))
        block.vector(run("dve"))
        block.gpsimd(run("pool"))
        block.sync(run("sp"))


def build_program():
    nc = bass.Bass("TRN2", target_bir_lowering=False)

    def din(name, shape, dt=F32):
        return nc.dram_tensor(name, list(shape), dt, kind="ExternalInput").ap()

    xw_own = din("xw_own", [1024, D])
    xw_oth = din("xw_oth", [1024, D])
    posb = din("posb", [1, 2048], I32)
    fake = din("fake", [128, 1])
    oh8d = din("oh8", [128, 8])
    ohbd = din("ohb", [128, 4])
    cT4 = din("cT4", [128, 64])
    w_ada_s = din("w_ada_s", [D, 1536])
    b_ada_s = din("b_ada_s", [1, 1536])
    gmT = din("gmT", [128, 16])
    gfT = din("gfT", [128, 16])
    gfin = din("gfin", [1, D])
    w_in = din("w_in", [D, 8720])
    b_in = din("b_in", [1, 8720])
    w_sq = din("w_sq", [D, 1024])
    w_sqr = din("w_sqr", [D, 1024])
    w_skr = din("w_skr", [D, 256])
    bcols = din("bcols", [128, 80])
    sinks = din("sinks", [1, 16])
    w_ba = din("w_ba", [1024, D])
    w_bb = din("w_bb", [1024, D])
    w_out = din("w_out", [D, D])
    w_r = din("w_r", [D, NE])
    b_r = din("b_r", [1, NE])
    b_dn = din("b_dn", [NE, D])
    if STAGE >= 8:
        w_gu = din("w_gu_l", [4, D, 2 * D])
        b_guT = din("b_guT_l", [128, 4 * 32])
        w_dn = din("w_dn_l", [4, D, D])
    out = nc.dram_tensor("out", [1024, D], F32, kind="ExternalOutput").ap()
    modg = nc.dram_tensor("modg", [4, 6 * D], F32).ap()
    moda = din("moda_dbg", [4, 6 * D]) if NOCC else nc.dram_tensor("moda", [4, 6 * D], F32).ap()
    if STAGE >= 8:
        xg = nc.dram_tensor("xg", [1024, 8192], F32)
        xga = nc.dram_tensor("xga", [1024, 8192], F32)
        gg = nc.dram_tensor("gg", [1024, 256], F32)
        gga = nc.dram_tensor("gga", [1024, 256], F32)
        hsd = nc.dram_tensor("hs", [1024, D], F32)
        pbuf = nc.dram_tensor("pbuf", [8192, D], F32)
        pall = nc.dram_tensor("pall", [8192, D], F32)

    C_BQ, C_BK, C_SQ, C_SQR, C_SK, C_SKR, C_GA, C_GB, C_BV = 0, 8, 16, 24, 32, 34, 36, 52, 68

    es = ExitStack()
    with es:
        def sb(name, shape, dt):
            return es.enter_context(nc.sbuf_tensor(name, list(shape), dt))

        def psum(name):
            return es.enter_context(nc.psum_tensor(name, [128, 512], F32))

        XA = sb("XA", [128, 16, 1024], BF16)
        XB = sb("XB", [128, 16, 1024], BF16)
        RH = sb("RH", [128, 8, 2048], F32)
        WBt = [sb("WB0", [128, 8192], BF16), sb("WB1", [128, 8192], BF16)]
        gt1b = sb("gt1b", [128, D], BF16)
        gt2b = sb("gt2b", [128, D], BF16)
        tab = sb("tab", [128, 4096], BF16)
        scr0 = sb("scr0", [128, D], BF16)
        pool4 = sb("pool4", [128, D], BF16)
        biasq = [sb("biasq%d" % i, [128, 16, 16], F32) for i in range(2)]
        bdn = sb("bdn", [128, D], BF16)
        GT = sb("GT", [128, 1024], BF16)
        ident = sb("ident", [128, 128], BF16)
        identf = sb("identf", [128, 128], F32)
        ones_f = sb("ones_f", [128, 128], F32)
        U_f = sb("U_f", [128, 128], F32)
        E64 = sb("E64", [128, 128], F32)
        maskC = sb("maskC", [128, 512], BF16)
        maskP = sb("maskP", [128, 512], BF16)
        zmask = sb("zmask", [128, 128], BF16)
        bcol = sb("bcol", [128, 80], F32)
        bgu = sb("bgu", [128, 4 * 32], F32)
        modcol = sb("modcol", [128, 96], F32)
        oh8 = sb("oh8_sb", [128, 8], F32)
        ohb = sb("ohb_sb", [128, 4], F32)
        cact4 = sb("cact4", [128, 64], F32)
        Gsel = sb("Gsel", [128, 8, 4], F32)
        g84 = sb("g84", [128, 8, 8, 4], F32)
        gs1 = sb("gs1", [128, 16], F32)
        gs2 = sb("gs2", [128, 16], F32)
        stat = sb("stat", [128, 64], F32)
        fakec = sb("fakec", [128, 1], F32)
        zcol = sb("zcol", [128, 1], F32)
        smallf = sb("smallf", [128, 64], F32)
        ipi = sb("ipi", [128, 4], I32)
        wf_t = sb("wf_t", [128, 16, 16], BF16)
        wr_t = sb("wr_t", [128, 16, NE], BF16)
        bsvb = sb("bsvb", [128, 256], F32)
        bfb = sb("bfb", [128, 16], F32)
        brb = sb("brb", [128, NE], F32)
        esink = sb("esink", [128, 16], F32)
        lf = sb("lf", [128, 16, 16], F32)
        negcum = sb("negcum", [128, 16, 16], F32)
        Cbn = sb("Cbn", [128, 8, 16], F32)
        G = sb("G", [128, 8, NE], F32)
        gb16 = sb("gb16", [128, NE], BF16)
        rt0 = sb("rt0", [128, NE], F32)
        rt1 = sb("rt1", [128, NE], F32)
        lg = sb("lg", [128, NE], F32)
        PS = [psum("ps%d" % i) for i in range(8)]

        S = Sched(nc)
        op = S.op

        WB = [t[:] for t in WBt]
        WBf = [t[:].bitcast(F32) for t in WBt]
        RHb = RH[:].rearrange("p a b -> p (a b)").bitcast(BF16)
        yaT = RHb[:, 0:8192].rearrange("p (c t) -> p c t", c=8)
        ybT = RHb[:, 8192:16384].rearrange("p (c t) -> p c t", c=8)
        WS = RHb[:, 16384:32768]
        XBf = XB[:].rearrange("p a b -> p (a b)").bitcast(F32)
        tabf = tab[:].bitcast(F32)
        cosT = tab[:, 0:2048]
        sinT = tab[:, 2048:4096]
        TABN = ["tab0", "tab1", "tab2", "tab3"]
        scr0f = scr0[:].bitcast(F32)
        TF = [(scr0f[:, 0:512], "scr0a"), (scr0f[:, 512:1024], "scr0b"),
              (tabf[:, 0:512], "tab0"), (tabf[:, 512:1024], "tab1"),
              (tabf[:, 1024:1536], "tab2"), (tabf[:, 1536:2048], "tab3")]
        SCR = [(scr0[:], ["scr0a", "scr0b"]), (pool4[:], ["sp0", "sp1", "sp2", "sp3a", "sp3b"])]
        PT = [(pool4[:, i * 512:(i + 1) * 512].rearrange("p (h q) -> p h q", h=4), "sp%d" % i) for i in range(3)]
        ytok = [(pool4[:, 1536:1792], "sp3a"), (pool4[:, 1792:2048], "sp3b")]
        TB = [(pool4[:, 0:512], ["sp0"]), (pool4[:, 512:1024], ["sp1"]), (pool4[:, 1024:1536], ["sp2"]),
              (pool4[:, 1536:2048], ["sp3a", "sp3b"])]
        brow = [(lf[0:1, :, :].rearrange("p a b -> p (a b)"), "lf"), (negcum[0:1, :, :].rearrange("p a b -> p (a b)"), "negcum")]
        iof = WBf[0][:, 0:512]
        W1T = [WBf[1][:, i * 512:(i + 1) * 512] for i in range(4)]
        posi = WBt[1][:].bitcast(I32)[:, 2048:2560]
        PSb = [p[:].bitcast(BF16) for p in PS]
        HN = ["h%d" % i for i in range(8)]

        def mm(reads, writes, lst):
            def fn(e, lst=lst):
                ins = None
                for (o, l, r, st, sp) in lst:
                    ins = e.matmul(o, lhsT=l, rhs=r, start=st, stop=sp)
                return ins
            return op("pe", fn, reads=reads, writes=writes)

        def dma(eng, dst, src, reads, writes):
            return op(eng, lambda e: e.dma_start(out=dst, in_=src), reads=reads, writes=writes, dma=True)

        def act(o, i, func, reads, writes, bias=None, scale=None, accum=None):
            kw = {}
            if bias is not None:
                kw["bias"] = bias
            if scale is not None:
                kw["scale"] = scale
            if accum is not None:
                kw["accum_out"] = accum
            return op("act", lambda e: e.activation(out=o, in_=i, func=func, **kw), reads=reads, writes=writes)

        def ts(o, i, s1, s2, op0, op1, reads, writes, eng="dve"):
            if op1 is None:
                return op(eng, lambda e: e.tensor_scalar(out=o, in0=i, scalar1=s1, scalar2=None, op0=op0),
                          reads=reads, writes=writes)
            return op(eng, lambda e: e.tensor_scalar(out=o, in0=i, scalar1=s1, scalar2=s2, op0=op0, op1=op1),
                      reads=reads, writes=writes)

        def tt(o, a, b, aop, reads, writes, eng="dve"):
            return op(eng, lambda e: e.tensor_tensor(out=o, in0=a, in1=b, op=aop), reads=reads, writes=writes)

        def stt(o, a, s, b, op0, op1, reads, writes, eng="dve"):
            return op(eng, lambda e: e.scalar_tensor_tensor(out=o, in0=a, scalar=s, in1=b, op0=op0, op1=op1),
                      reads=reads, writes=writes)

        def cp(o, i, reads, writes, eng="dve"):
            return op(eng, lambda e: e.tensor_copy(out=o, in_=i), reads=reads, writes=writes)

        def allreduce8_stages(a_, b_, a_res, b_res):
            g1 = [[0, 1, 2, 3], [4, 5, 6, 7]]
            g2 = [[0, 4], [1, 5], [2, 6], [3, 7]]

            def s1():
                op("pool", lambda e: e.collective_compute("AllReduce", ALU.add, replica_groups=g1,
                                                          ins=[a_.opt()], outs=[b_.opt()]),
                   reads=a_res, writes=[b_res], cc=True)

            def s2():
                op("pool", lambda e: e.collective_compute("AllReduce", ALU.add, replica_groups=g2,
                                                          ins=[b_.opt()], outs=[a_.opt()]),
                   reads=[b_res], writes=a_res, cc=True)
            return [s1, s2]

        def allreduce8(a_, b_, a_res, b_res):
            for f_ in allreduce8_stages(a_, b_, a_res, b_res):
                f_()

        op("dve", lambda e: e.memset(stat[:], 0.0), writes=["stat"])
        op("pool", lambda e: e.iota(iof.rearrange("p (a b) -> p a b", a=4), pattern=[[0, 4], [1, 128]], base=0,
                                     channel_multiplier=-1, allow_small_or_imprecise_dtypes=True), writes=["WB0"])
        ts(identf[:], iof[:, 0:128], 0.0, None, ALU.is_equal, None, ["WB0"], ["identf"])
        cp(ident[:], identf[:], ["identf"], ["ident"])
        ts(U_f[:], iof[:, 0:128], 0.0, None, ALU.is_ge, None, ["WB0"], ["U_f"])
        ts(maskC[:], iof, 0.0, NEG, ALU.is_lt, ALU.mult, ["WB0"], ["maskC"])
        ts(maskP[:], iof, 0.0, NEG, ALU.is_ge, ALU.mult, ["WB0"], ["maskP"])
        op("dve", lambda e: e.memset(ones_f[:], 1.0), writes=["ones_f"])
        op("dve", lambda e: e.memset(zcol[:], 0.0), writes=["zcol"])
        op("dve", lambda e: e.memset(zmask[:], 0.0), writes=["zmask"])
        op("dve", lambda e: e.memset(GT[:], 0.0), writes=["GT"])
        op("dve", lambda e: e.memset(bdn[:], 0.0), writes=["bdn"])
        op("pool", lambda e: e.iota(E64[:], pattern=[[0, 128]], base=-64, channel_multiplier=1,
                                     allow_small_or_imprecise_dtypes=True), writes=["E64"])
        ts(E64[:], E64[:], 0.0, None, ALU.is_equal, None, ["E64"], ["E64"])
        dma("sp", fakec[:], fake, [], ["fakec"])
        dma("sp", bcol[:], bcols, [], ["bcol"])
        dma("sp", bsvb[:], b_in[0:1, 1280:1536].partition_broadcast(128), [], ["bsvb"])
        dma("sp", bfb[:], b_in[0:1, FG:FG + 16].partition_broadcast(128), [], ["bfb"])
        dma("sp", brb[:], b_r.partition_broadcast(128), [], ["brb"])
        dma("sp", esink[:], sinks.partition_broadcast(128), [], ["esink"])
        act(esink[:], esink[:], AF.Exp, ["esink"], ["esink"])
        dma("sp", oh8[:], oh8d, [], ["oh8"])
        dma("sp", ohb[:], ohbd, [], ["ohb"])
        dma("sp", cact4[:], cT4, [], ["cact4"])
        act(cact4[:], cact4[:], AF.Silu, ["cact4"], ["cact4"])
        dma("sp", smallf[:, 16:32], gmT, [], ["gmTl"])
        dma("sp", smallf[:, 32:48], gfT, [], ["gfTl"])

        op("pool", lambda e: e.iota(ipi[:, 0:1], pattern=[[0, 1]], base=0, channel_multiplier=1), writes=["ipi"])
        op("dve", lambda e: e.tensor_single_scalar(out=ipi[:, 1:2], in_=ipi[:, 0:1], scalar=31, op=ALU.bitwise_and),
           reads=["ipi"], writes=["ipi1"])
        op("dve", lambda e: e.tensor_single_scalar(out=ipi[:, 2:3], in_=ipi[:, 0:1], scalar=32, op=ALU.bitwise_and),
           reads=["ipi"], writes=["ipi2"])
        cp(smallf[:, 48:50], ipi[:, 1:3], ["ipi1", "ipi2"], ["ipf"])
        act(smallf[:, 50:51], smallf[:, 48:49], AF.Exp, ["ipf"], ["invf"], scale=-float(np.log(10000.0)) / 32.0)
        ts(smallf[:, 50:51], smallf[:, 50:51], float(1.0 / (2 * np.pi)), None, ALU.mult, None, ["invf"], ["invf"])
        ts(smallf[:, 51:52], smallf[:, 49:50], 1.0 / 16.0, -1.0, ALU.mult, ALU.add, ["ipf"], ["sgn"])
        for q4 in range(4):
            tsl = slice(q4 * 512, (q4 + 1) * 512)
            dma("sp", posi, posb[0:1, tsl].partition_broadcast(128), [], ["WB1"])
            cp(W1T[0], posi, ["WB1"], ["WB1"])
            ts(W1T[1], W1T[0], smallf[:, 50:51], None, ALU.mult, None, ["WB1", "invf"], ["WB1"])
            for which, dst, off in (("s", sinT, 0.0), ("c", cosT, 0.25)):
                ts(W1T[2], W1T[1], off, None, ALU.add, None, ["WB1"], ["WB1"])
                cp(posi, W1T[2], ["WB1"], ["WB1"])
                cp(W1T[3], posi, ["WB1"], ["WB1"])
                tt(W1T[2], W1T[2], W1T[3], ALU.subtract, ["WB1"], ["WB1"])
                act(W1T[3], W1T[2], AF.Sin, ["WB1"], ["WB1"], scale=float(2 * np.pi))
                if which == "s":
                    ts(dst[:, tsl], W1T[3], smallf[:, 51:52], None, ALU.mult, None, ["WB1", "sgn"], TABN)
                else:
                    cp(dst[:, tsl], W1T[3], ["WB1"], TABN)

        RHf = RH[:].rearrange("p a b -> p (a b)")
        RHN = ["yaT", "ybT", "WS"]
        c4v = cact4[:].rearrange("p (k b) -> p k b", b=4)
        w_ada_v = w_ada_s.rearrange("(k p) c -> p k c", p=128)
        mods = RHf[0:4, 0:1536]
        for j in range(6):
            b = j % 2
            wv = WBf[b].rearrange("p (k c) -> p k c", k=16)
            br_ap, br_n = brow[b]
            dma("sp", wv, w_ada_v[:, :, j * 256:(j + 1) * 256], [], ["WB%d" % b])
            dma("sp", br_ap, b_ada_s[0:1, j * 256:(j + 1) * 256], [], [br_n])
            pr = PS[1 + b]
            prn = "ps%d" % (1 + b)
            lst = [(pr[0:4, 0:256], c4v[:, k, :], wv[:, k, :], k == 0, False) for k in range(16)]
            lst.append((pr[0:4, 0:256], ones_f[0:1, 0:4], br_ap[0:1, :], False, True))
            mm(["WB%d" % b, br_n, "cact4", "ones_f"], [prn], lst)
            cp(mods[:, j * 256:(j + 1) * 256], pr[0:4, 0:256], [prn], RHN)
        expd = RHf[0:4, 2048:2048 + 12288].rearrange("p (s c) -> p s c", s=8)
        tt(expd, mods.unsqueeze(1).broadcast_to([4, 8, 1536]), oh8[0:4, :].unsqueeze(2).broadcast_to([4, 8, 1536]),
           ALU.mult, RHN + ["oh8"], RHN)
        dma("sp", modg, RHf[0:4, 2048:2048 + 12288], RHN, ["modg"])
        if not NOCC:
            allreduce8(modg, moda, ["modg"], "moda")
        mod_src, mod_res = (moda, []) if NOCC else (modg, ["modg"])
        mrow = RHf[0:96, 0:512].rearrange("p (b q) -> p b q", b=4)
        dma("sp", mrow, mod_src.rearrange("b (j q) -> j b q", q=128), mod_res, RHN)
        mmine = RHf[0:96, 512:640]
        ts(mmine, mrow[:, 0, :], ohb[0:96, 0:1], None, ALU.mult, None, RHN + ["ohb"], RHN)
        for bb in range(1, 4):
            stt(mmine, mrow[:, bb, :], ohb[0:96, bb:bb + 1], mmine, ALU.mult, ALU.add, RHN + ["ohb"], RHN)
        op("pe", lambda e: e.transpose(out=PS[0][:, 0:96], in_=mmine, identity=identf[0:96, 0:96]),
           reads=RHN + ["identf"], writes=["ps0"])
        cp(modcol[:], PS[0][:, 0:96], ["ps0"], ["modcol"])
        pidx = RHf[0:96, 640:768]
        op("pool", lambda e: e.iota(pidx, pattern=[[0, 128]], base=0, channel_multiplier=1,
                                     allow_small_or_imprecise_dtypes=True), reads=[], writes=RHN)
        for jj in range(32):
            j0 = (32 + jj) if jj < 16 else (80 + jj - 16)
            sel = RHf[0:96, 768 + (jj % 2) * 128:896 + (jj % 2) * 128]
            ts(sel, pidx, float(j0), None, ALU.is_equal, None, RHN, RHN)
            bk = 1 + (jj % 2)
            mm(RHN, ["ps%d" % bk], [(PS[bk][:, 0:128], sel, mmine, True, True)])
            dstt = (gt1b if jj < 16 else gt2b)[:, (jj % 16) * 128:(jj % 16 + 1) * 128]
            act(dstt, PS[bk][:, 0:128], AF.Copy, ["ps%d" % bk], ["gt1b" if jj < 16 else "gt2b"])
        SH1, SC1, SH2, SC2 = modcol[:, 0:16], modcol[:, 16:32], modcol[:, 48:64], modcol[:, 64:80]
        stt(gs1[:], SC1, 1.0, smallf[:, 16:32], ALU.add, ALU.mult, ["modcol", "gmTl"], ["gs1"])
        stt(gs2[:], SC2, 1.0, smallf[:, 32:48], ALU.add, ALU.mult, ["modcol", "gfTl"], ["gs2"])

        ev_toggle = [0]

        def norm_block(src_ap, src_res, bi, dstX, dst_res, idx, gs, sh_i, statcol):
            sc, scn = SCR[bi % 2]
            stn = "st%d" % statcol
            ssq = stat[:, statcol:statcol + 1]
            act(sc, src_ap, AF.Square, [src_res, "stat"], scn + [stn], accum=ssq)
            ts(ssq, ssq, 1.0 / D, 1e-5, ALU.mult, ALU.add, [stn], [stn])
            act(ssq, ssq, AF.Sqrt, [stn], [stn])
            op("dve", lambda e: e.reciprocal(out=ssq, in_=ssq), reads=[stn], writes=[stn])
            ts(sc, src_ap, ssq, None, ALU.mult, None, [src_res, stn], scn)
            banks = (2, 3) if bi % 2 == 0 else (4, 5)
            for hb in range(2):
                bk = banks[hb]

                def fn(e, bk=bk, hb=hb):
                    ins = None
                    for kk in range(8):
                        k = hb * 8 + kk
                        ins = e.transpose(out=PSb[bk][:, kk * 128:(kk + 1) * 128], in_=sc[:, k * 128:(k + 1) * 128],
                                          identity=ident[:])
                    return ins
                op("pe", fn, reads=scn + ["ident"], writes=["ps%d" % bk])
                for kk in range(8):
                    k = hb * 8 + kk
                    d = dstX[:, k, idx * 128:(idx + 1) * 128]
                    s_ = PSb[bk][:, kk * 128:(kk + 1) * 128]
                    ev_toggle[0] ^= 1
                    if ev_toggle[0]:
                        act(d, s_, AF.Identity, ["ps%d" % bk, "gs1", "gs2", "modcol"], [dst_res],
                            bias=sh_i[:, k:k + 1], scale=gs[:, k:k + 1])
                    else:
                        ts(d, s_, gs[:, k:k + 1], sh_i[:, k:k + 1], ALU.mult, ALU.add,
                           ["ps%d" % bk, "gs1", "gs2", "modcol"], [dst_res])

        for bi in range(16):
            own = bi % 2 == 0
            idx = bi // 2
            src = (xw_own if own else xw_oth)[idx * 128:(idx + 1) * 128, :]
            b = bi % 2
            xb = WBf[b][:, 0:2048]
            dma("sp", xb, src, [], ["WB%d" % b])
            norm_block(xb, "WB%d" % b, bi, XA if own else XB, "XA" if own else "XB", idx, gs1, SH1, bi)

        def Xtok(kidx):
            X = XA if kidx < 8 else XB
            i = kidx % 8
            return X, ("XA" if kidx < 8 else "XB"), slice(i * 128, (i + 1) * 128)

        def kidx_of_slot(s):
            return (s // 2) if s % 2 == 1 else 8 + s // 2

        proj_rr = [0]

        def proj_bank():
            proj_rr[0] = (proj_rr[0] + 1) % 4
            return proj_rr[0]

        def load_w(b, views):
            for d_, s_ in views:
                dma("pool", d_, s_, [], ["WB%d" % b])

        w_in_v = w_in.rearrange("(k p) c -> p k c", p=128)

        if STAGE >= 2:
            dma("pool", wf_t[:], w_in_v[:, :, FG:FG + 16], [], ["wf_t"])
            pfb = PS[6][:, 0:256].rearrange("p (s h) -> p s h", s=16)
            for kidx in range(16):
                X, xn_, tsl = Xtok(kidx)
                mm([xn_, "wf_t"], ["ps6"], [(pfb[:, kidx, :], X[:, k, tsl], wf_t[:, k, :], k == 0, k == 15) for k in range(16)])
            tt(lf[:], pfb, bfb[:, :].unsqueeze(1).broadcast_to([128, 16, 16]), ALU.add, ["ps6", "bfb"], ["lf"])
            act(lf[:], lf[:], AF.Sigmoid, ["lf"], ["lf"])
            act(lf[:], lf[:], AF.Ln, ["lf"], ["lf"])
            pcm = PS[7][:, 0:256].rearrange("p (s h) -> p s h", s=16)
            lst = []
            for s in range(16):
                ks = kidx_of_slot(s)
                for m_ in range(s):
                    lst.append((pcm[:, ks, :], ones_f[:], lf[:, kidx_of_slot(m_), :], m_ == 0, False))
                lst.append((pcm[:, ks, :], U_f[:], lf[:, ks, :], s == 0, True))
            mm(["lf", "ones_f", "U_f"], ["ps7"], lst)
            ts(negcum[:], pcm, -1.0, None, ALU.mult, None, ["ps7"], ["negcum"])
            pcb = PS[0][:, 0:128].rearrange("p (i h) -> p i h", i=8)
            mm(["negcum", "E64"], ["ps0"], [(pcb[:, i, :], E64[:], negcum[:, i, :], True, True) for i in range(8)])
            cp(Cbn[:], pcb, ["ps0"], ["Cbn"])
            ts(negcum[:, 8, :], negcum[:, 8, :], fakec[:, 0:1], None, ALU.add, None, ["negcum", "fakec"], ["negcum"])

        att_rr = [0]
        acc_rr = [0]
        pt_rr = [0]
        yt_rr = [0]

        ACCB = [6, 7, 2, 3]
        tr_rr = [0]

        def attention_unit(i, units, KT, QT_of, V, vres, kres, qres, bias_of, bias_res, mask_of, nq, sink_cols, yT, yres,
                           chunk0, ybias):
            accs = [PS[ACCB[h]][:, 0:65] for h in range(4)]
            accn = ["ps%d" % ACCB[h] for h in range(4)]
            nj = len(units)
            for ji, j in enumerate(units):
                att_rr[0] ^= 1
                sbk = 4 + att_rr[0]
                psv = PS[sbk][:].rearrange("p (h q) -> p h q", h=4)
                lst = []
                if nq == 4:
                    for h4 in range(4):
                        mk = mask_of(j)
                        mk_ap = mk[:, 0:128] if mk is not None else zmask[:]
                        lst.append((psv[:, h4, :], ident[:], mk_ap, True, False))
                        lst.append((psv[:, h4, :], KT(j, h4), QT_of(h4), False, True))
                else:
                    mk = mask_of(j)
                    lst.append((PS[sbk][:], ident[:], mk[:], True, False))
                    lst.append((psv, KT(j, 0), QT_of(0), False, True))
                mm([kres, qres, "ident", "maskC", "maskP", "zmask"], ["ps%d" % sbk], lst)
                pt_rr[0] = (pt_rr[0] + 1) % 3
                pt, ptn = PT[pt_rr[0]]
                if nq == 4:
                    for h4 in range(4):
                        act(pt[:, h4, :], psv[:, h4, :], AF.Exp, ["ps%d" % sbk] + bias_res, [ptn],
                            bias=bias_of(j, h4), scale=0.125)
                else:
                    act(pt, psv, AF.Exp, ["ps%d" % sbk] + bias_res, [ptn], bias=bias_of(j, 0), scale=0.125)
                mm([ptn, vres], accn,
                   [(accs[h4], pt[:, h4, :], V(j, h4), ji == 0, ji == nj - 1) for h4 in range(4)])
            den = smallf[:, 56:60]
            for h4 in range(4):
                if sink_cols is not None:
                    tt(den[:, h4:h4 + 1], accs[h4][:, 64:65], sink_cols[:, h4:h4 + 1], ALU.add, [accn[h4], "esink"], ["den"])
                else:
                    cp(den[:, h4:h4 + 1], accs[h4][:, 64:65], [accn[h4]], ["den"])
            op("dve", lambda e: e.reciprocal(out=den, in_=den), reads=["den"], writes=["den"])
            yt_rr[0] ^= 1
            yt, ytn = ytok[yt_rr[0]]
            for h4 in range(4):
                ts(yt[:, h4 * 64:(h4 + 1) * 64], accs[h4][:, 0:64], den[:, h4:h4 + 1], None, ALU.mult, None,
                   [accn[h4], "den"], [ytn])
            tr_rr[0] ^= 1
            tb_ = tr_rr[0]

            def fn(e):
                ins = None
                for c2 in range(2):
                    ins = e.transpose(out=PSb[tb_][:, c2 * 128:(c2 + 1) * 128], in_=yt[:, c2 * 128:(c2 + 1) * 128],
                                      identity=ident[:])
                return ins
            op("pe", fn, reads=[ytn, "ident"], writes=["ps%d" % tb_])
            if ybias is None:
                act(yT[:, chunk0:chunk0 + 2, i * 128:(i + 1) * 128],
                    PSb[tb_][:, 0:256].rearrange("p (c q) -> p c q", c=2), AF.Copy, ["ps%d" % tb_], [yres])
            else:
                for c2 in range(2):
                    act(yT[:, chunk0 + c2, i * 128:(i + 1) * 128], PSb[tb_][:, c2 * 128:(c2 + 1) * 128], AF.Identity,
                        ["ps%d" % tb_, "bcol"], [yres], bias=bcol[:, ybias + c2:ybias + c2 + 1])

        if STAGE >= 3:
            KTs = WS[:, 0:4096].rearrange("p (c t) -> p c t", c=2)
            QTs = WS[:, 4096:12288].rearrange("p (c t) -> p c t", c=8)
            Vs = RHb[:, 8192:8192 + 4160].rearrange("p (s h c) -> p s h c", s=16, h=4)
            op("dve", lambda e: e.memset(Vs[:, :, :, 64:65], 1.0), writes=["ybT"])
            w_sq_v = w_sq.rearrange("(k p) c -> p k c", p=128)
            w_sqr_v = w_sqr.rearrange("(k p) c -> p k c", p=128)
            w_skr_v = w_skr.rearrange("(k p) c -> p k c", p=128)

            def rope_evac(bkA, bkB, colA, colB, tabsl, dst, dres):
                (ta, tan), (tb2, tbn) = TF[0], TF[1]
                stt(ta, PS[bkA][:], bcol[:, colA:colA + 1], cosT[:, tabsl], ALU.add, ALU.mult,
                    ["ps%d" % bkA, "bcol"] + TABN, [tan])
                stt(tb2, PS[bkB][:], bcol[:, colB:colB + 1], sinT[:, tabsl], ALU.add, ALU.mult,
                    ["ps%d" % bkB, "bcol"] + TABN, [tbn])
                tt(dst, ta, tb2, ALU.add, [tan, tbn], [dres])

            wv = WB[0].rearrange("p (k c) -> p k c", k=16)
            load_w(0, [(wv[:, :, 0:256], w_in_v[:, :, 1024:1280]), (wv[:, :, 256:512], w_skr_v[:, :, :])])
            for kp in range(2):
                for t4 in range(4):
                    X = XA if t4 < 2 else XB
                    xr = "XA" if t4 < 2 else "XB"
                    tsl = slice((t4 % 2) * 512, (t4 % 2 + 1) * 512)
                    gsl = slice(t4 * 512, (t4 + 1) * 512)
                    bA, bB = proj_bank(), proj_bank()
                    mm(["WB0", xr], ["ps%d" % bA], [(PS[bA][:], wv[:, k, kp * 128:(kp + 1) * 128], X[:, k, tsl], k == 0, k == 15) for k in range(16)])
                    mm(["WB0", xr], ["ps%d" % bB], [(PS[bB][:], wv[:, k, 256 + kp * 128:256 + (kp + 1) * 128], X[:, k, tsl], k == 0, k == 15) for k in range(16)])
                    rope_evac(bA, bB, C_SK + kp, C_SKR + kp, gsl, KTs[:, kp, gsl], "WS")
            wv1 = WB[1].rearrange("p (k c) -> p k c", k=16)
            load_w(1, [(wv1[:, :, 0:256], w_in_v[:, :, 1280:1536])])
            for kidx in range(16):
                X, xr, tsl = Xtok(kidx)
                bk = proj_bank()
                mm(["WB1", xr], ["ps%d" % bk], [(PS[bk][:, 0:256], X[:, k, tsl], wv1[:, k, 0:256], k == 0, k == 15) for k in range(16)])
                tt(Vs[:, kidx, :, 0:64], PS[bk][:, 0:256].rearrange("p (h c) -> p h c", h=4),
                   bsvb[:].rearrange("p (h c) -> p h c", h=4), ALU.add, ["ps%d" % bk, "bsvb"], ["ybT"])
            for m4 in range(4):
                b = m4 % 2
                wq = WB[b].rearrange("p (k c) -> p k c", k=16)
                load_w(b, [(wq[:, :, 0:256], w_sq_v[:, :, m4 * 256:(m4 + 1) * 256]),
                           (wq[:, :, 256:512], w_sqr_v[:, :, m4 * 256:(m4 + 1) * 256])])
                for c2 in range(2):
                    c = m4 * 2 + c2
                    for t2 in range(2):
                        tsl = slice(t2 * 512, (t2 + 1) * 512)
                        bA, bB = proj_bank(), proj_bank()
                        mm(["WB%d" % b, "XA"], ["ps%d" % bA], [(PS[bA][:], wq[:, k, c2 * 128:(c2 + 1) * 128], XA[:, k, tsl], k == 0, k == 15) for k in range(16)])
                        mm(["WB%d" % b, "XA"], ["ps%d" % bB], [(PS[bB][:], wq[:, k, 256 + c2 * 128:256 + (c2 + 1) * 128], XA[:, k, tsl], k == 0, k == 15) for k in range(16)])
                        rope_evac(bA, bB, C_SQ + c, C_SQR + c, tsl, QTs[:, c, tsl], "WS")
            for i in range(8):
                for kvh in range(4):
                    hh, kp = kvh % 2, kvh // 2
                    hs = slice(hh * 64, (hh + 1) * 64)
                    attention_unit(
                        i, [8 + i, i],
                        KT=lambda j, h4, hs=hs, kp=kp: KTs[hs, kp, j * 128:(j + 1) * 128],
                        QT_of=lambda h4, hs=hs, kp=kp, i=i: QTs[hs, kp * 4:(kp + 1) * 4, i * 128:(i + 1) * 128],
                        V=lambda j, h4, kvh=kvh: Vs[:, j, kvh, :],
                        vres="ybT", kres="WS", qres="WS",
                        bias_of=lambda j, h4, i=i: (fakec[:, 0:1] if (i == 0 and j == 8) else zcol[:, 0:1]),
                        bias_res=["fakec", "zcol"],
                        mask_of=lambda j: (maskP if j >= 8 else maskC),
                        nq=1, sink_cols=esink[:, 4 * kvh:4 * kvh + 4], yT=yaT, yres="yaT", chunk0=2 * kvh, ybias=None)

        if STAGE >= 4:
            KTg = WS[:, 0:4096].rearrange("p (c t) -> p c t", c=2)
            QTg = WS[:, 4096:6144].rearrange("p (c t) -> p c t", c=2)
            Vg = WS[:, 6144:6144 + 4160].rearrange("p (s h c) -> p s h c", s=16, h=4)
            for g in range(int(os.environ.get('MK_FOXG', '4'))):
                wqk = WB[0].rearrange("p (k c) -> p k c", k=16)
                load_w(0, [(wqk[:, :, 0:256], w_in_v[:, :, 1536 + 256 * g:1536 + 256 * (g + 1)]),
                           (wqk[:, :, 256:512], w_in_v[:, :, 2560 + 256 * g:2560 + 256 * (g + 1)])])
                wv1 = WB[1].rearrange("p (k c) -> p k c", k=16)
                load_w(1, [(wv1[:, :, 0:256], w_in_v[:, :, 3584 + 256 * g:3584 + 256 * (g + 1)])])
                for pr in range(2):
                    for t4 in range(4):
                        X = XA if t4 < 2 else XB
                        xr = "XA" if t4 < 2 else "XB"
                        tsl = slice((t4 % 2) * 512, (t4 % 2 + 1) * 512)
                        gsl = slice(t4 * 512, (t4 + 1) * 512)
                        bk = proj_bank()
                        mm(["WB0", xr], ["ps%d" % bk], [(PS[bk][:], wqk[:, k, 256 + pr * 128:256 + (pr + 1) * 128], X[:, k, tsl], k == 0, k == 15) for k in range(16)])
                        act(KTg[:, pr, gsl], PS[bk][:], AF.Identity, ["ps%d" % bk, "bcol"], ["WS"],
                            bias=bcol[:, C_BK + 2 * g + pr:C_BK + 2 * g + pr + 1])
                    for t2 in range(2):
                        tsl = slice(t2 * 512, (t2 + 1) * 512)
                        bk = proj_bank()
                        mm(["WB0", "XA"], ["ps%d" % bk], [(PS[bk][:], wqk[:, k, pr * 128:(pr + 1) * 128], XA[:, k, tsl], k == 0, k == 15) for k in range(16)])
                        act(QTg[:, pr, tsl], PS[bk][:], AF.Identity, ["ps%d" % bk, "bcol"], ["WS"],
                            bias=bcol[:, C_BQ + 2 * g + pr:C_BQ + 2 * g + pr + 1])
                op("dve", lambda e: e.memset(Vg[:, :, :, 64:65], 1.0), writes=["WS"])
                for kidx in range(16):
                    X, xr, tsl = Xtok(kidx)
                    bk = proj_bank()
                    mm(["WB1", xr], ["ps%d" % bk], [(PS[bk][:, 0:256], X[:, k, tsl], wv1[:, k, 0:256], k == 0, k == 15) for k in range(16)])
                    cp(Vg[:, kidx, :, 0:64], PS[bk][:, 0:256].rearrange("p (h c) -> p h c", h=4), ["ps%d" % bk], ["WS"])
                for i in range(int(os.environ.get('MK_FOXI', '8'))):
                    bq, bqn = biasq[i % 2], "biasq%d" % (i % 2)
                    tt(bq[:], negcum[:], Cbn[:, i, :].unsqueeze(1).broadcast_to([128, 16, 16]), ALU.subtract,
                       ["negcum", "Cbn"], [bqn])
                    units = [8 + m_ for m_ in range(i + 1)] + list(range(i + 1))
                    attention_unit(
                        i, units,
                        KT=lambda j, h4: KTg[(h4 % 2) * 64:(h4 % 2 + 1) * 64, h4 // 2, j * 128:(j + 1) * 128],
                        QT_of=lambda h4, i=i: QTg[(h4 % 2) * 64:(h4 % 2 + 1) * 64, h4 // 2, i * 128:(i + 1) * 128],
                        V=lambda j, h4: Vg[:, j, h4, :],
                        vres="WS", kres="WS", qres="WS",
                        bias_of=lambda j, h4, bq=bq, g=g: bq[:, j, 4 * g + h4:4 * g + h4 + 1],
                        bias_res=[bqn],
                        mask_of=lambda j, i=i: (maskC if j == i else None),
                        nq=4, sink_cols=None, yT=ybT, yres="ybT", chunk0=2 * g, ybias=C_BV + 2 * g)

        mergedT = XB
        if STAGE >= 5:
            w_ba_v = w_ba.rearrange("(k p) c -> p k c", p=128)
            w_bb_v = w_bb.rearrange("(k p) c -> p k c", p=128)
            for dc in range(16):
                b = dc % 2
                wa = WB[b][:, 0:1024].rearrange("p (k c) -> p k c", k=8)
                wb_ = WB[b][:, 1024:2048].rearrange("p (k c) -> p k c", k=8)
                wga = WB[b][:, 2048:4096].rearrange("p (k c) -> p k c", k=16)
                wgb = WB[b][:, 4096:6144].rearrange("p (k c) -> p k c", k=16)
                cs = slice(dc * 128, (dc + 1) * 128)
                load_w(b, [(wa, w_ba_v[:, :, cs]), (wb_, w_bb_v[:, :, cs]),
                           (wga, w_in_v[:, :, GA0 + dc * 128:GA0 + (dc + 1) * 128]),
                           (wgb, w_in_v[:, :, GB0 + dc * 128:GB0 + (dc + 1) * 128])])
                for t2 in range(2):
                    tsl = slice(t2 * 512, (t2 + 1) * 512)
                    u = (dc * 2 + t2) % 2
                    base = 4 * u
                    bGA, bGB, bA, bB = base, base + 1, base + 2, base + 3
                    wn = "WB%d" % b
                    mm([wn, "XA"], ["ps%d" % bGA], [(PS[bGA][:], wga[:, k, :], XA[:, k, tsl], k == 0, k == 15) for k in range(16)])
                    mm([wn, "XA"], ["ps%d" % bGB], [(PS[bGB][:], wgb[:, k, :], XA[:, k, tsl], k == 0, k == 15) for k in range(16)])
                    mm([wn, "yaT"], ["ps%d" % bA], [(PS[bA][:], wa[:, k, :], yaT[:, k, tsl], k == 0, k == 7) for k in range(8)])
                    mm([wn, "ybT"], ["ps%d" % bB], [(PS[bB][:], wb_[:, k, :], ybT[:, k, tsl], k == 0, k == 7) for k in range(8)])
                    (sA, sAn), (sB, sBn) = TB[2 * u], TB[2 * u + 1]
                    act(sA, PS[bGA][:], AF.Sigmoid, ["ps%d" % bGA, "bcol"], sAn, bias=bcol[:, C_GA + dc:C_GA + dc + 1])
                    act(sB, PS[bGB][:], AF.Sigmoid, ["ps%d" % bGB, "bcol"], sBn, bias=bcol[:, C_GB + dc:C_GB + dc + 1])
                    (t1, t1n), (t2_, t2n) = TF[2 * u], TF[2 * u + 1]
                    tt(t1, PS[bA][:], sA, ALU.mult, ["ps%d" % bA] + sAn, [t1n])
                    tt(t2_, PS[bB][:], sB, ALU.mult, ["ps%d" % bB] + sBn, [t2n])
                    tt(mergedT[:, dc, tsl], t1, t2_, ALU.add, [t1n, t2n], ["XB"])

        if STAGE >= 6:
            for tb in range(8):
                dma("sp", RH[:, tb, :], xw_own[tb * 128:(tb + 1) * 128, :], [], [HN[tb], "yaT", "ybT", "WS"])
            w_out_v = w_out.rearrange("(k p) c -> p k c", p=128)
            for dq in range(4):
                b = dq % 2
                wn = "WB%d" % b
                wv = WB[b].rearrange("p (k c) -> p k c", k=16)
                cs = slice(dq * 512, (dq + 1) * 512)
                load_w(b, [(wv, w_out_v[:, :, cs])])
                tt(wv, wv, gt1b[:, cs].unsqueeze(1).broadcast_to([128, 16, 512]), ALU.mult, [wn, "gt1b"], [wn])
                for tb in range(8):
                    bk = proj_bank()
                    mm([wn, "XB"], ["ps%d" % bk], [(PS[bk][:], mergedT[:, k, tb * 128:(tb + 1) * 128], wv[:, k, :], k == 0, k == 15) for k in range(16)])
                    tt(RH[:, tb, cs], RH[:, tb, cs], PS[bk][:], ALU.add, [HN[tb], "ps%d" % bk], [HN[tb]])

        if STAGE >= 7:
            dma("pool", wr_t[:], w_r.rearrange("(k p) c -> p k c", p=128), [], ["wr_t"])
            dma("pool", bdn[0:NE, :], b_dn, [], ["bdn"])
            tt(bdn[0:NE, :], bdn[0:NE, :], gt2b[0:NE, :], ALU.mult, ["bdn", "gt2b"], ["bdn"])
            for tb in range(8):
                norm_block(RH[:, tb, :], HN[tb], tb, XA, "XA", tb, gs2, SH2, 16 + tb)
                tsl = slice(tb * 128, (tb + 1) * 128)
                bk = proj_bank()
                mm(["XA", "wr_t"], ["ps%d" % bk], [(PS[bk][:, 0:NE], XA[:, k, tsl], wr_t[:, k, :], k == 0, k == 15) for k in range(16)])
                tt(lg[:], PS[bk][:, 0:NE], brb[:], ALU.add, ["ps%d" % bk, "brb"], ["lg"])
                m8 = smallf[:, 0:8]
                op("dve", lambda e: e.max(out=m8, in_=lg[:]), reads=["lg"], writes=["m8"])
                ts(smallf[:, 8:9], smallf[:, 0:1], -1.0, None, ALU.mult, None, ["m8"], ["nmx"])
                act(rt0[:], lg[:], AF.Exp, ["lg", "nmx"], ["rt0"], bias=smallf[:, 8:9])
                ts(rt1[:], lg[:], smallf[:, 3:4], None, ALU.is_ge, None, ["lg", "m8"], ["rt1"])
                tt(rt0[:], rt0[:], rt1[:], ALU.mult, ["rt0", "rt1"], ["rt0"])
                op("dve", lambda e: e.reduce_sum(out=smallf[:, 9:10], in_=rt0[:], axis=AX.X), reads=["rt0"], writes=["gden"])
                op("dve", lambda e: e.reciprocal(out=smallf[:, 9:10], in_=smallf[:, 9:10]), reads=["gden"], writes=["gden"])
                ts(G[:, tb, :], rt0[:], smallf[:, 9:10], None, ALU.mult, None, ["rt0", "gden"], ["G"])
                cp(gb16[:], G[:, tb, :], ["G"], ["gb16"])
                bk2 = proj_bank()
                op("pe", lambda e, bk2=bk2: e.transpose(out=PSb[bk2][0:NE, 0:128], in_=gb16[:], identity=ident[:]),
                   reads=["gb16", "ident"], writes=["ps%d" % bk2])
                act(GT[0:NE, tsl], PSb[bk2][0:NE, 0:128], AF.Copy, ["ps%d" % bk2], ["GT"])
            for tb in range(8):
                for dq in range(4):
                    cs = slice(dq * 512, (dq + 1) * 512)
                    bk = proj_bank()
                    mm(["GT", "bdn"], ["ps%d" % bk], [(PS[bk][:], GT[:, tb * 128:(tb + 1) * 128], bdn[:, cs], True, True)])
                    tt(RH[:, tb, cs], RH[:, tb, cs], PS[bk][:], ALU.add, [HN[tb], "ps%d" % bk], [HN[tb]])

        if STAGE >= 8:
            actT = XB
            XAw = XA[:].rearrange("p a b -> p (a b)").bitcast(F32)
            XBw = XB[:].rearrange("p a b -> p (a b)").bitcast(F32)
            XAflat = XA[:].rearrange("p a b -> p (a b)")
            XBflat = XB[:].rearrange("p a b -> p (a b)")
            dma("sp", bgu[:], b_guT, [], ["bgu"])
            dma("sp", hsd.ap().rearrange("(t p) d -> p t d", p=128), RH[:], HN, ["hs"])
            ggt = tabf[:, 0:2048].rearrange("p (s c) -> p s c", s=8)
            tt(ggt, G[:].rearrange("p a b -> p (a b)").unsqueeze(1).broadcast_to([128, 8, 256]),
               oh8[:].unsqueeze(2).broadcast_to([128, 8, 256]), ALU.mult, ["G", "oh8"], TABN)
            dma("sp", gg.ap().rearrange("(s p) c -> p s c", p=128), ggt, TABN, ["gg"])
            RHflat = RH[:].rearrange("p a b -> p (a b)")
            for s8 in range(8):
                hb = s8 % 2
                stg = RHflat[:, hb * 8192:(hb + 1) * 8192]
                hn = HN[hb * 4:hb * 4 + 4]
                ts(stg.bitcast(BF16), XAflat, oh8[:, s8:s8 + 1], None, ALU.mult, None, ["XA", "oh8"], hn)
                dma("sp", xg.ap()[s8 * 128:(s8 + 1) * 128, :], stg, hn, ["xg%d" % s8])
            allreduce8(gg.ap(), gga.ap(), ["gg"], "gga")
            pending = []

            def xg_stages(s8):
                return allreduce8_stages(xg.ap()[s8 * 128:(s8 + 1) * 128, :], xga.ap()[s8 * 128:(s8 + 1) * 128, :],
                                         ["xg%d" % s8], "xga%d" % s8)

            def emit_pending(n=1):
                for _ in range(n):
                    if pending:
                        pending.pop(0)()
            for f_ in xg_stages(0):
                f_()
            wb_rr = [0]
            up_rr = [0]
            dn_rr = [0]
            n_ch = int(os.environ.get("MK_NCH", "8"))
            for c8 in range(n_ch):
                if c8 + 1 < n_ch:
                    pending[0:0] = xg_stages(c8 + 1)
                dma("sp", XAw, xg.ap()[c8 * 128:(c8 + 1) * 128, :], ["xg%d" % c8], ["XA"])
                dma("sp", G[:].rearrange("p a b -> p (a b)"), gg.ap()[c8 * 128:(c8 + 1) * 128, :], ["gg"], ["G"])
                tt(g84[:], G[:].rearrange("p t (r l) -> p t r l", r=8),
                   oh8[:].unsqueeze(1).unsqueeze(3).broadcast_to([128, 8, 8, 4]), ALU.mult, ["G", "oh8"], ["g84"])
                op("dve", lambda e: e.reduce_sum(out=Gsel[:], in_=g84[:].rearrange("p t r l -> p t l r"), axis=AX.X),
                   reads=["g84"], writes=["Gsel"])
                for le in range(4):
                    for p8 in range(8):
                        b = wb_rr[0]
                        wb_rr[0] ^= 1
                        wn = "WB%d" % b
                        wv = WB[b].rearrange("p (k two c) -> p k two c", k=16, two=2)
                        srcv = w_gu[le].rearrange("(k p) c -> p k c", p=128)
                        load_w(b, [(wv[:, :, 0, :], srcv[:, :, p8 * 256:(p8 + 1) * 256]),
                                   (wv[:, :, 1, :], srcv[:, :, 2048 + p8 * 256:2048 + (p8 + 1) * 256])])
                        for fc in range(2):
                            ch = p8 * 2 + fc
                            for t2 in range(2):
                                tsl = slice(t2 * 512, (t2 + 1) * 512)
                                up_rr[0] = (up_rr[0] + 1) % 3
                                bG, bL = 2 * up_rr[0], 2 * up_rr[0] + 1
                                mm([wn, "XA"], ["ps%d" % bG], [(PS[bG][:], wv[:, k, 0, fc * 128:(fc + 1) * 128], XA[:, k, tsl], k == 0, k == 15) for k in range(16)])
                                mm([wn, "XA"], ["ps%d" % bL], [(PS[bL][:], wv[:, k, 1, fc * 128:(fc + 1) * 128], XA[:, k, tsl], k == 0, k == 15) for k in range(16)])
                                u = (ch * 2 + t2) % 2
                                (tg, tgn), (tl, tln), (tsg, tsn) = TF[3 * u], TF[3 * u + 1], TF[3 * u + 2]
                                bgc = bgu[:, le * 32 + ch:le * 32 + ch + 1]
                                blc = bgu[:, le * 32 + 16 + ch:le * 32 + 16 + ch + 1]
                                ts(tg, PS[bG][:], bgc, 7.0, ALU.add, ALU.min, ["ps%d" % bG, "bgu"], [tgn])
                                act(tsg, tg, AF.Sigmoid, [tgn], [tsn], scale=1.702)
                                ts(tl, PS[bL][:], blc, 7.0, ALU.add, ALU.min, ["ps%d" % bL, "bgu"], [tln])
                                ts(tl, tl, -7.0, 1.0, ALU.max, ALU.add, [tln], [tln])
                                tt(tg, tg, tsg, ALU.mult, [tgn, tsn], [tgn])
                                tt(actT[:, ch, tsl], tg, tl, ALU.mult, [tgn, tln], ["XB"])
                    emit_pending(1)
                    for q4 in range(4):
                        b = wb_rr[0]
                        wb_rr[0] ^= 1
                        wn = "WB%d" % b
                        wv = WB[b].rearrange("p (k c) -> p k c", k=16)
                        cs = slice(q4 * 512, (q4 + 1) * 512)
                        if q4 == 2:
                            emit_pending(1)
                        load_w(b, [(wv, w_dn[le].rearrange("(k p) c -> p k c", p=128)[:, :, cs])])
                        for tb in range(8):
                            dn_rr[0] ^= 1
                            bk = 6 + dn_rr[0]
                            mm([wn, "XB"], ["ps%d" % bk], [(PS[bk][:], actT[:, k, tb * 128:(tb + 1) * 128], wv[:, k, :], k == 0, k == 15) for k in range(16)])
                            if le == 0:
                                ts(RH[:, tb, cs], PS[bk][:], Gsel[:, tb, 0:1], None, ALU.mult, None,
                                   ["ps%d" % bk, "Gsel", "hs"], [HN[tb]])
                            else:
                                stt(RH[:, tb, cs], PS[bk][:], Gsel[:, tb, le:le + 1], RH[:, tb, cs], ALU.mult, ALU.add,
                                    ["ps%d" % bk, "Gsel", HN[tb]], [HN[tb]])
                for tb in range(8):
                    dma("sp", pbuf.ap()[c8 * 1024 + tb * 128:c8 * 1024 + (tb + 1) * 128, :], RH[:, tb, :], [HN[tb]],
                        ["pb%d_%d" % (c8, tb)])
                st_ = []
                for hf in range(2):
                    r0 = c8 * 1024 + hf * 512
                    st_.append(allreduce8_stages(pbuf.ap()[r0:r0 + 512, :], pall.ap()[r0:r0 + 512, :],
                                                 ["pb%d_%d" % (c8, tb) for tb in range(hf * 4, hf * 4 + 4)], "pall%d_%d" % (c8, hf)))
                pending.extend([st_[0][0], st_[1][0], st_[0][1], st_[1][1]])
            emit_pending(len(pending))
            XAq = XAw.rearrange("p (s d) -> p s d", s=4)
            XBq = XBw.rearrange("p (s d) -> p s d", s=4)
            pall_v = pbuf.ap().rearrange("(s t p) d -> t p s d", s=8, p=128)
            macc = tabf[:, 0:2048]
            for tb in range(8):
                PBN = ["pb%d_%d" % (s_, tb) for s_ in range(n_ch)]
                dma("sp", RH[:, tb, :], hsd.ap()[tb * 128:(tb + 1) * 128, :], ["hs"], [HN[tb]])
                dma("sp", XAq, pall_v[tb, :, 0:4, :], PBN, ["XA"])
                dma("sp", XBq, pall_v[tb, :, 4:8, :], PBN, ["XB"])
                ts(macc, XAq[:, 0, :], oh8[:, 0:1], None, ALU.mult, None, ["XA", "oh8"], TABN)
                for s8 in range(1, 8):
                    srcq = XAq[:, s8, :] if s8 < 4 else XBq[:, s8 - 4, :]
                    stt(macc, srcq, oh8[:, s8:s8 + 1], macc, ALU.mult, ALU.add, ["XA", "XB", "oh8"] + TABN, TABN)
                tt(macc, macc, gt2b[:], ALU.mult, TABN + ["gt2b"], TABN)
                tt(RH[:, tb, :], RH[:, tb, :], macc, ALU.add, [HN[tb]] + TABN, [HN[tb]])

        if STAGE >= 6:
            if STAGE >= 9:
                gfb = WBf[0][:, 0:2048]
                dma("sp", gfb, gfin.partition_broadcast(128), [], ["WB0"])
            for tb in range(8):
                if STAGE >= 9:
                    col = 32 + tb
                    stn = "st%d" % col
                    ssq = stat[:, col:col + 1]
                    sc, scn = SCR[tb % 2]
                    act(sc, RH[:, tb, :], AF.Square, [HN[tb], "stat"], scn + [stn], accum=ssq)
                    ts(ssq, ssq, 1.0 / D, 1e-5, ALU.mult, ALU.add, [stn], [stn])
                    act(ssq, ssq, AF.Sqrt, [stn], [stn])
                    op("dve", lambda e, ssq=ssq: e.reciprocal(out=ssq, in_=ssq), reads=[stn], writes=[stn])
                    stt(RH[:, tb, :], RH[:, tb, :], ssq, gfb, ALU.mult, ALU.mult, [HN[tb], stn, "WB0"], [HN[tb]])
                dma("sp", out[tb * 128:(tb + 1) * 128, :], RH[:, tb, :], [HN[tb]], [])
        else:
            dbg = {1: ("XA", XA[:].rearrange("p a b -> p (a b)").bitcast(F32)),
                   2: ("negcum", negcum[:].rearrange("p a b -> p (a b)")),
                   3: ("yaT", RH[:].rearrange("p a b -> p (a b)")[:, 0:4096]),
                   4: ("ybT", RH[:].rearrange("p a b -> p (a b)")[:, 4096:8192]),
                   5: ("XB", XB[:].rearrange("p a b -> p (a b)").bitcast(F32))}[STAGE]
            n = dbg[1].shape[1]
            if n >= 2048:
                dma("sp", out.rearrange("(a p) c -> p a c", p=128)[:, 0:(n // 2048), :],
                    dbg[1].rearrange("p (a c) -> p a c", c=2048), [dbg[0], "gs1", "modcol", "negcum", "lf", "Cbn"], [])
            else:
                dma("sp", out[0:128, 0:n], dbg[1], [dbg[0], "negcum", "lf", "Cbn"], [])

        S.finish("sp", [(("dma", i), S.dma_use[i] * 16) for i in range(S.n_dma) if S.dma_use[i]])
        S.emit(es)
    return nc


_PROGRAM = None


def _prep(inputs):
    f = lambda a: np.ascontiguousarray(a, dtype=np.float32)
    x = f(inputs["x"]); c = f(inputs["c"]); pos = np.ascontiguousarray(inputs["positions"]).astype(np.int32)
    w_in = f(inputs["w_in"][0]); b_in = f(inputs["b_in"][0])
    colT = lambda v: np.ascontiguousarray(v.reshape(-1, 128).T)
    order = []
    for cch in range(8):
        for half in range(2):
            qh = 8 * (cch // 4) + 4 * half + cch % 4
            order.extend(range(qh * 64, (qh + 1) * 64))
    order = np.array(order)
    rot = lambda cols: (cols // 64) * 64 + (cols % 64 + 32) % 64
    w_sq = np.ascontiguousarray(w_in[:, order]); w_sqr = np.ascontiguousarray(w_in[:, rot(order)])
    kcols = 1024 + np.arange(256)
    krot = 1024 + rot(np.arange(256))
    w_skr = np.ascontiguousarray(w_in[:, krot])
    bcols = np.zeros((128, 80), np.float32)
    bcols[:, 0:8] = colT(b_in[0 + 1536:1536 + 1024])
    bcols[:, 8:16] = colT(b_in[2560:2560 + 1024])
    bcols[:, 16:24] = colT(b_in[order]); bcols[:, 24:32] = colT(b_in[rot(order)])
    bcols[:, 32:34] = colT(b_in[kcols]); bcols[:, 34:36] = colT(b_in[krot])
    bcols[:, 36:52] = colT(b_in[GA0:GA0 + 2048]); bcols[:, 52:68] = colT(b_in[GB0:GB0 + 2048])
    bcols[:, 68:76] = colT(b_in[3584:4608])
    b_gu = f(inputs["b_gu"][0])
    w_ada = f(inputs["w_ada"][0]); b_ada = f(inputs["b_ada"][0]).reshape(1, -1)
    w_gu = f(inputs["w_gu"][0]); w_dn = f(inputs["w_down"][0])
    cT4 = np.ascontiguousarray(c.reshape(4, 16, 128).transpose(2, 1, 0).reshape(128, 64))
    shared = {
        "gmT": colT(f(inputs["g_mix"][0])), "gfT": colT(f(inputs["g_ffn"][0])),
        "gfin": f(inputs["g_final"]).reshape(1, -1), "cT4": cT4,
        "w_in": w_in, "b_in": b_in.reshape(1, -1), "w_sq": w_sq, "w_sqr": w_sqr, "w_skr": w_skr,
        "bcols": bcols, "sinks": f(inputs["sinks"][0]).reshape(1, 16),
        "w_ba": f(inputs["w_branch_a"][0]), "w_bb": f(inputs["w_branch_b"][0]), "w_out": f(inputs["w_out"][0]),
        "w_r": f(inputs["w_router"][0]), "b_r": f(inputs["b_router"][0]).reshape(1, -1),
        "b_dn": f(inputs["b_down"][0]),
    }
    maps = []
    for cid in range(8):
        b, p = cid // 2, cid % 2
        if p == 1:
            xw = x[b]; pw = pos[b]
        else:
            xw = np.concatenate([np.zeros((128, D), np.float32), x[b, :1920]], 0)
            pw = np.concatenate([np.zeros((128,), np.int32), pos[b, :1920]], 0)
        xw = xw.reshape(8, 2, 128, D); pw = pw.reshape(8, 2, 128)
        m = dict(shared)
        m["xw_own"] = np.ascontiguousarray(xw[:, 1].reshape(1024, D))
        m["xw_oth"] = np.ascontiguousarray(xw[:, 0].reshape(1024, D))
        m["posb"] = np.ascontiguousarray(np.concatenate([pw[:, 1].reshape(-1), pw[:, 0].reshape(-1)])[None, :])
        m["fake"] = np.full((128, 1), NEG if p == 0 else 0.0, np.float32)
        oh8 = np.zeros((128, 8), np.float32); oh8[:, cid] = 1.0
        ohb = np.zeros((128, 4), np.float32); ohb[:, b] = 1.0
        m["oh8"] = oh8; m["ohb"] = ohb
        if NOCC:
            cf = c.astype(np.float64)
            m["moda_dbg"] = ((cf / (1 + np.exp(-cf))) @ w_ada.astype(np.float64) + b_ada).astype(np.float32)
        m["w_ada_s"] = np.ascontiguousarray(w_ada[:, cid * 1536:(cid + 1) * 1536])
        m["b_ada_s"] = np.ascontiguousarray(b_ada[:, cid * 1536:(cid + 1) * 1536])
        if STAGE >= 8:
            m["w_gu_l"] = w_gu[4 * cid:4 * cid + 4]
            m["w_dn_l"] = w_dn[4 * cid:4 * cid + 4]
            m["b_guT_l"] = np.ascontiguousarray(
                b_gu[4 * cid:4 * cid + 4].reshape(4, 32, 128).transpose(2, 0, 1).reshape(128, 4 * 32))
        maps.append(m)
    return maps


def kernel(**inputs):
    global _PROGRAM
    if _PROGRAM is None:
        _PROGRAM = build_program()
    maps = _prep(inputs)
    res = run_bass_kernel_spmd(_PROGRAM, maps, core_ids=list(range(8)))
    outf = np.zeros((4, 2048, D), np.float32)
    o4 = outf.reshape(4, 8, 2, 128, D)
    for cid in range(8):
        b, p = cid // 2, cid % 2
        o = np.asarray(res.results[cid]["out"]).reshape(8, 128, D)
        o4[b, :, p] = o
    return outf
```
